# Optimizing a Trainium2 kernel written in Bass

```python
import math, functools
import jax, jax.numpy as jnp
from jax import lax
import numpy as np

D_MODEL = 4096
BATCH = 1
SEQ = 8192
DEPTH = 4

HA = 16
DKA = 128
DVA = 128
CONV_K = 4
CHUNK = 64
HB = 32
HB_KV = 8
DHB = 64
WINDOW = 128
BLK = 128
N_BUCKETS = 32
MAX_DIST = WINDOW
N_EXPERTS = 32
TOP_K = 4
D_EXPERT = 256
SWIGLU_LIMIT = 7.0
SWIGLU_ALPHA = 1.702
DN_ALPHA = (2 * DEPTH) ** 0.25
DN_BETA = (8 * DEPTH) ** -0.25
LN_EPS = 1e-5
NORM_EPS = 1e-6
N_MOD = 6
WA = HA * DKA
WVA = HA * DVA
WQB = HB * DHB
WKVB = HB_KV * DHB
_SPLITS = (WA, WA, WVA, WVA, HA, HA, WQB, WKVB, WKVB, D_MODEL, D_MODEL)
C_IN = WA * 2 + WVA * 2 + HA * 2 + WQB + WKVB * 2 + D_MODEL * 2

kernel_name = "hybrid_deltanet_swa_moe_deepnorm"


def _layernorm(x, g, b):
    xf = x.astype(jnp.float32)
    mu = xf.mean(-1, keepdims=True)
    var = jnp.square(xf - mu).mean(-1, keepdims=True)
    return ((xf - mu) * lax.rsqrt(var + LN_EPS) * g + b).astype(x.dtype)


def _l2norm(x):
    return x * lax.rsqrt(jnp.sum(x * x, -1, keepdims=True) + NORM_EPS)


def _causal_conv_silu(x, w):
    y = lax.conv_general_dilated(x, w[:, None, :].astype(x.dtype), window_strides=(1,),
                                 padding=((CONV_K - 1, 0),),
                                 dimension_numbers=('NHC', 'HIO', 'NHC'),
                                 feature_group_count=x.shape[-1])
    return jax.nn.silu(y)


def _gated_delta_rule(q, k, v, beta, g):
    B, S, H, Dk = q.shape
    Dv = v.shape[-1]
    N = S // CHUNK

    def chunks(t):
        return jnp.moveaxis(t.reshape((B, N, CHUNK, H) + t.shape[3:]), 3, 1)

    q, k, v, beta, g = map(chunks, (q, k, v, beta, g))
    G = jnp.cumsum(g, axis=-1)
    causal = jnp.tril(jnp.ones((CHUNK, CHUNK), bool))
    strict = jnp.tril(jnp.ones((CHUNK, CHUNK), bool), -1)
    decay = jnp.exp(jnp.where(causal, G[..., :, None] - G[..., None, :], -jnp.inf))
    kb = k * beta[..., None]
    L = jnp.where(strict, jnp.einsum('bhncd,bhnsd->bhncs', kb, k) * decay, 0.0)
    A = L + jnp.eye(CHUNK, dtype=L.dtype)
    solve = functools.partial(lax.linalg.triangular_solve, left_side=True, lower=True,
                              unit_diagonal=True)
    u = solve(A, v * beta[..., None])
    w = solve(A, kb * jnp.exp(G)[..., None])
    intra = jnp.einsum('bhncd,bhnsd->bhncs', q, k) * decay
    q_dec = q * jnp.exp(G)[..., None]
    G_last = G[..., -1]
    k_dec = k * jnp.exp(G_last[..., None] - G)[..., None]

    def step(state, xs):
        u_n, w_n, intra_n, q_n, k_n, gl_n = xs
        v_new = u_n - jnp.einsum('bhcd,bhde->bhce', w_n, state)
        o_n = (jnp.einsum('bhcd,bhde->bhce', q_n, state)
               + jnp.einsum('bhcs,bhse->bhce', intra_n, v_new))
        state = state * jnp.exp(gl_n)[..., None, None] + jnp.einsum('bhcd,bhce->bhde', k_n, v_new)
        return state, o_n

    xs = tuple(jnp.moveaxis(t, 2, 0) for t in (u, w, intra, q_dec, k_dec, G_last))
    s0 = jnp.zeros((B, H, Dk, Dv), jnp.float32)
    _, o = lax.scan(step, s0, xs)
    return o.transpose(1, 0, 3, 2, 4).reshape(B, S, H, Dv)


def _t5_bucket(d):
    n_exact = N_BUCKETS // 2
    df = jnp.maximum(d, 1).astype(jnp.float32)
    large = n_exact + (jnp.log(df / n_exact) / math.log(MAX_DIST / n_exact)
                       * (N_BUCKETS - n_exact)).astype(jnp.int32)
    large = jnp.minimum(large, N_BUCKETS - 1)
    return jnp.where(d < n_exact, d, large)


def _band_bias_and_mask(rel_bias, n_blocks):
    i = jnp.arange(BLK)[:, None]
    j = jnp.arange(2 * BLK)[None, :]
    d = i + BLK - j
    key_pos = jnp.arange(n_blocks)[:, None, None] * BLK - BLK + j
    valid = (d >= 0) & (d < WINDOW) & (key_pos >= 0)
    bucket = _t5_bucket(jnp.clip(d, 0, MAX_DIST - 1))
    bias = rel_bias.astype(jnp.float32)[bucket]
    bias = bias.transpose(2, 0, 1).reshape(HB_KV, HB // HB_KV, BLK, 2 * BLK)
    return bias, valid


def _swa_sinks(q, k, v, sinks, bias, valid):
    B, S = q.shape[:2]
    NB = S // BLK
    G = HB // HB_KV
    qb = q.reshape(B, NB, BLK, HB_KV, G, DHB)

    def band(t):
        tp = jnp.pad(t, ((0, 0), (BLK, 0), (0, 0), (0, 0))).reshape(B, NB + 1, BLK, HB_KV, DHB)
        return jnp.concatenate([tp[:, :-1], tp[:, 1:]], axis=2)

    kk, vv = band(k), band(v)
    s = jnp.einsum('bnqkgd,bnskd->bnkgqs', qb, kk).astype(jnp.float32) * (DHB ** -0.5) + bias
    s = jnp.where(valid[None, :, None, None], s, -jnp.inf)
    sink = jnp.broadcast_to(sinks.astype(jnp.float32).reshape(HB_KV, G, 1, 1), s.shape[:-1] + (1,))
    p = jax.nn.softmax(jnp.concatenate([s, sink], axis=-1), axis=-1)[..., :-1].astype(v.dtype)
    o = jnp.einsum('bnkgqs,bnskd->bnqkgd', p, vv)
    return o.reshape(B, S, HB * DHB)


def _mixer(u, w_in, conv_w, a_log, dt_bias, norm_a, sinks, w_up_a, w_up_b, w_o, bias, valid):
    B, S, _ = u.shape
    f32 = jnp.float32
    proj = u @ w_in
    qA, kA, vA, zA, bA, aA, qB, kB, vB, gA, gB = jnp.split(
        proj, [int(t) for t in np.cumsum(_SPLITS)[:-1]], axis=-1)
    qkv = _causal_conv_silu(jnp.concatenate([qA, kA, vA], axis=-1), conv_w)
    qA, kA, vA = jnp.split(qkv, [WA, 2 * WA], axis=-1)
    qA = _l2norm(qA.reshape(B, S, HA, DKA).astype(f32)) * (DKA ** -0.5)
    kA = _l2norm(kA.reshape(B, S, HA, DKA).astype(f32))
    vA = vA.reshape(B, S, HA, DVA).astype(f32)
    beta = jax.nn.sigmoid(bA.astype(f32))
    g = -jnp.exp(a_log.astype(f32)) * jax.nn.softplus(aA.astype(f32) + dt_bias.astype(f32))
    oA = _gated_delta_rule(qA, kA, vA, beta, g)
    oA = (oA * lax.rsqrt(jnp.mean(oA * oA, -1, keepdims=True) + NORM_EPS) * norm_a.astype(f32)
          * jax.nn.silu(zA.reshape(B, S, HA, DVA).astype(f32)))
    oA = oA.reshape(B, S, WVA).astype(u.dtype)
    oB = _swa_sinks(qB.reshape(B, S, HB, DHB), kB.reshape(B, S, HB_KV, DHB),
                    vB.reshape(B, S, HB_KV, DHB), sinks, bias, valid)
    merged = jax.nn.sigmoid(gA) * (oA @ w_up_a) + jax.nn.sigmoid(gB) * (oB @ w_up_b)
    return merged @ w_o


def _moe(u, w_router, b_router, w_gate_up, b_gate_up, w_down, b_down):
    f32 = jnp.float32
    logits = (u @ w_router).astype(f32) + b_router.astype(f32)
    top_val, top_idx = lax.top_k(logits, TOP_K)
    top_w = jax.nn.softmax(top_val, axis=-1)
    comb = jnp.sum(jax.nn.one_hot(top_idx, N_EXPERTS, dtype=f32) * top_w[..., None],
                   axis=-2).astype(u.dtype)
    gu = jnp.einsum('bsd,edf->bsef', u, w_gate_up) + b_gate_up
    gate, up = jnp.split(gu, 2, axis=-1)
    gate = jnp.minimum(gate, SWIGLU_LIMIT)
    up = jnp.clip(up, -SWIGLU_LIMIT, SWIGLU_LIMIT)
    h = (up + 1.0) * gate * jax.nn.sigmoid(SWIGLU_ALPHA * gate)
    return jnp.einsum('bsef,efd->bsd', h * comb[..., None], w_down) + comb @ b_down


def setup_inputs(seed: int = 0) -> dict:
    key = jax.random.key(seed)
    ks = jax.random.split(key, 26)
    f32 = jnp.float32

    def nrm(k, shape, scale):
        return jax.random.normal(k, shape, f32) * scale

    L = DEPTH
    dt = jnp.exp(jax.random.uniform(ks[8], (L, HA), f32, math.log(1e-3), math.log(1e-1)))
    return {
        "x": nrm(ks[0], (BATCH, SEQ, D_MODEL), 1.0),
        "c": nrm(ks[1], (BATCH, D_MODEL), 1.0),
        "w_ada": nrm(ks[2], (D_MODEL, N_MOD * D_MODEL), 0.5 * D_MODEL ** -0.5),
        "b_ada": nrm(ks[3], (N_MOD * D_MODEL,), 0.01),
        "ada_table": nrm(ks[4], (L, N_MOD, D_MODEL), 0.02),
        "rel_bias": nrm(ks[5], (N_BUCKETS, HB), 0.5),
        "w_in": nrm(ks[6], (L, D_MODEL, C_IN), D_MODEL ** -0.5),
        "conv_w": nrm(ks[7], (L, CONV_K, 3 * WA), CONV_K ** -0.5),
        "a_log": jnp.log(jax.random.uniform(ks[9], (L, HA), f32, 1.0, 16.0)),
        "dt_bias": jnp.log(jnp.expm1(dt)),
        "norm_a": 1.0 + nrm(ks[10], (L, DVA), 0.01),
        "sinks": nrm(ks[11], (L, HB), 1.0),
        "w_up_a": nrm(ks[12], (L, WVA, D_MODEL), WVA ** -0.5),
        "w_up_b": nrm(ks[13], (L, WQB, D_MODEL), WQB ** -0.5),
        "w_o": nrm(ks[14], (L, D_MODEL, D_MODEL), DN_BETA * D_MODEL ** -0.5),
        "ln1_g": 1.0 + nrm(ks[15], (L, D_MODEL), 0.01),
        "ln1_b": nrm(ks[16], (L, D_MODEL), 0.01),
        "w_router": nrm(ks[17], (L, D_MODEL, N_EXPERTS), D_MODEL ** -0.5),
        "b_router": nrm(ks[18], (L, N_EXPERTS), 0.01),
        "w_gate_up": nrm(ks[19], (L, N_EXPERTS, D_MODEL, 2 * D_EXPERT), D_MODEL ** -0.5),
        "b_gate_up": nrm(ks[20], (L, N_EXPERTS, 2 * D_EXPERT), 0.01),
        "w_down": nrm(ks[21], (L, N_EXPERTS, D_EXPERT, D_MODEL), DN_BETA * D_EXPERT ** -0.5),
        "b_down": nrm(ks[22], (L, N_EXPERTS, D_MODEL), 0.01),
        "ln2_g": 1.0 + nrm(ks[23], (L, D_MODEL), 0.01),
        "ln2_b": nrm(ks[24], (L, D_MODEL), 0.01),
    }


def reference(x, c, w_ada, b_ada, ada_table, rel_bias, w_in, conv_w, a_log, dt_bias, norm_a,
              sinks, w_up_a, w_up_b, w_o, ln1_g, ln1_b, w_router, b_router, w_gate_up,
              b_gate_up, w_down, b_down, ln2_g, ln2_b):
    B, S, D = x.shape
    mod_base = (jax.nn.silu(c) @ w_ada + b_ada).reshape(B, N_MOD, D)
    bias, valid = _band_bias_and_mask(rel_bias, S // BLK)
    for l in range(DEPTH):
        mod = (mod_base + ada_table[l])[:, :, None, :]
        sh1, sc1, gt1, sh2, sc2, gt2 = jnp.moveaxis(mod, 1, 0)
        u = x * (1.0 + sc1) + sh1
        y = _mixer(u, w_in[l], conv_w[l], a_log[l], dt_bias[l], norm_a[l], sinks[l],
                   w_up_a[l], w_up_b[l], w_o[l], bias, valid)
        x = _layernorm(DN_ALPHA * x + gt1 * y, ln1_g[l], ln1_b[l])
        u = x * (1.0 + sc2) + sh2
        y = _moe(u, w_router[l], b_router[l], w_gate_up[l], b_gate_up[l], w_down[l], b_down[l])
        x = _layernorm(DN_ALPHA * x + gt2 * y, ln2_g[l], ln2_b[l])
    return x
```

```python
import numpy as np
from contextlib import ExitStack
from concourse.bass_utils import run_bass_kernel_spmd
import concourse.bass as bass
import concourse.mybir as mybir

F32 = mybir.dt.float32
BF16 = mybir.dt.bfloat16
AF = mybir.ActivationFunctionType
ALU = mybir.AluOpType
AX = mybir.AxisListType

STRICT_SAME_ENGINE = True


class Buf:
    __slots__ = ("name", "last_w", "readers", "sem")

    def __init__(self, name):
        self.name = name
        self.last_w = None
        self.readers = []
        self.sem = None


class Prog:
    ENGS = ("pe", "act", "dve", "pool", "sp")

    def __init__(self, nc):
        self.nc = nc
        self.ins = []
        self.dma_sem_count = {}
        self.sems = []
        self._sem_ctx = []

    _uid = [0]

    def new_sem(self, name):
        Prog._uid[0] += 1
        s = self.nc.alloc_semaphore(name=f"{name}_{Prog._uid[0]}")
        self.sems.append(s)
        return s

    def release(self):
        nc = self.nc
        nc.all_engine_barrier()
        nc.clear_and_free_semaphores(self.sems)
        nc.all_engine_barrier()
        self.sems = []

    def buf(self, name):
        return Buf(name)

    def op(self, eng, fn, reads=(), writes=(), dma=False):
        i = len(self.ins)
        deps = set()
        for b in reads:
            if b.last_w is not None:
                deps.add(b.last_w)
        for b in writes:
            if b.last_w is not None:
                deps.add(b.last_w)
            deps.update(b.readers)
        tok = None
        if dma:
            b0 = writes[0]
            if b0.sem is None:
                b0.sem = self.new_sem("d_" + b0.name)
                self.dma_sem_count[b0.sem] = 0
            self.dma_sem_count[b0.sem] += 16
            tok = (b0.sem, self.dma_sem_count[b0.sem])
        self.ins.append([eng, fn, deps, dma, tok, False])
        for b in reads:
            b.readers.append(i)
        for b in writes:
            b.last_w = i
            b.readers = []
        return i

    def dma(self, q, out, in_, reads, writes, **kw):
        return self.op(q, lambda e: e.dma_start(out=out, in_=in_, **kw), reads, writes, dma=True)

    def mm(self, out, lhsT, rhs, start, stop, reads, writes, **kw):
        return self.op("pe", lambda e: e.matmul(out, lhsT, rhs, start=start, stop=stop, **kw), reads, writes)

    def emit(self):
        nc = self.nc
        ins = self.ins
        for i, (eng, fn, deps, dma, tok, sig) in enumerate(ins):
            for d in deps:
                p = ins[d]
                if p[3]:
                    continue
                if p[0] != eng or (STRICT_SAME_ENGINE and eng != "pe"):
                    p[5] = True
        esem = {e: self.new_sem("e_" + e) for e in self.ENGS}
        cnt = {e: 0 for e in self.ENGS}
        for it in ins:
            if not it[3] and it[5]:
                cnt[it[0]] += 1
                it[4] = (esem[it[0]], cnt[it[0]])
        per = {e: [] for e in self.ENGS}
        for i, it in enumerate(ins):
            per[it[0]].append(i)

        def run(e, lst):
            waited = {}
            for i in lst:
                eng, fn, deps, dma, tok, sig = ins[i]
                need = {}
                for d in deps:
                    t = ins[d][4]
                    if t is None:
                        continue
                    s, v = t
                    if waited.get(s, 0) >= v:
                        continue
                    if need.get(s, 0) < v:
                        need[s] = v
                for s, v in need.items():
                    e.wait_ge(s, v)
                    waited[s] = v
                h = fn(e)
                if dma:
                    h.then_inc(tok[0], 16)
                elif sig:
                    h.then_inc(tok[0], 1)

        with nc.Block() as block:
            @block.tensor
            def _(e):
                run(e, per["pe"])

            @block.scalar
            def _(e):
                run(e, per["act"])

            @block.vector
            def _(e):
                run(e, per["dve"])

            @block.gpsimd
            def _(e):
                run(e, per["pool"])

            @block.sync
            def _(e):
                run(e, per["sp"])
                for s, v in self.dma_sem_count.items():
                    e.wait_ge(s, v)
                for en in self.ENGS:
                    if en != "sp" and cnt[en] > 0:
                        e.wait_ge(esem[en], cnt[en])


D = 4096
KC = 32
NCORE = 8
HA, DKA, DVA = 16, 128, 128
HB, HBKV, DHB = 32, 8, 64
WA = HA * DKA
C_IN = 19488
OFF_QA, OFF_KA, OFF_VA, OFF_ZA = 0, 2048, 4096, 6144
OFF_BA, OFF_AA = 8192, 8208
OFF_QB, OFF_KB, OFF_VB = 8224, 10272, 10784
OFF_GA, OFF_GB = 11296, 15392
NE, DE = 32, 256
DN_ALPHA = 8.0 ** 0.25
LN_EPS = 1e-5
NORM_EPS = 1e-6


class Ctx:
    def __init__(self, nc):
        self.nc = nc
        self.P = Prog(nc)
        self.es = ExitStack()
        self.n = 0

    _gn = [0]

    def sb(self, shape, dt, name=None):
        Ctx._gn[0] += 1
        t = self.es.enter_context(self.nc.sbuf_tensor(f"{name or 's'}_{Ctx._gn[0]}", list(shape), dt))
        return t

    def ps(self, shape, dt=F32, name=None):
        Ctx._gn[0] += 1
        return self.es.enter_context(self.nc.psum_tensor(f"{name or 'p'}_{Ctx._gn[0]}", list(shape), dt))

    def buf(self, name):
        return self.P.buf(name)

    def finish(self):
        self.P.emit()
        self.es.close()
        self.P.release()


class Ring:
    def __init__(self, items):
        self.items = items
        self.i = 0

    def next(self):
        it = self.items[self.i % len(self.items)]
        self.i += 1
        return it


def sb_ring(cx, n, shape, dt, name):
    return Ring([(cx.sb(shape, dt, name), cx.buf(f"{name}{i}")) for i in range(n)])


def ps_ring(cx, n, shape, name, dt=F32):
    return Ring([(cx.ps(shape, dt, name), cx.buf(f"{name}{i}")) for i in range(n)])


def phase_adaln(nc, c_pk, w_sh, b_sh, out, ncols):
    cx = Ctx(nc)
    P = cx.P
    ct = cx.sb([128, KC], F32, "c")
    cs = cx.sb([128, KC], F32, "cs")
    bt = cx.sb([1, ncols], F32, "b")
    ot = cx.sb([1, ncols], F32, "o")
    Bc, Bcs, Bb, Bo, Bout = [cx.buf(n) for n in ("c", "cs", "b", "o", "out")]
    P.dma("sp", ct[:], c_pk, [], [Bc])
    P.dma("sp", bt[:], b_sh, [], [Bb])
    P.op("act", lambda e: e.activation(out=cs[:], in_=ct[:], func=AF.Silu), [Bc], [Bcs])
    nb = ncols // 512
    pss = [(cx.ps([1, 512], F32, "acc"), cx.buf(f"acc{i}")) for i in range(nb)]
    wr = sb_ring(cx, 3, [128, ncols], F32, "w")
    for kc in range(KC):
        wt, Bw = wr.next()
        P.dma("sp", wt[:], w_sh[kc * 128:(kc + 1) * 128, :], [], [Bw])
        for j in range(nb):
            ps, Bp = pss[j]
            P.mm(ps[:], cs[:, kc:kc + 1], wt[:, j * 512:(j + 1) * 512], kc == 0, kc == KC - 1, [Bcs, Bw], [Bp])
    for j in range(nb):
        ps, Bp = pss[j]
        P.op("dve", lambda e, ps=ps, j=j: e.tensor_tensor(out=ot[:, j * 512:(j + 1) * 512], in0=ps[:],
                                                         in1=bt[:, j * 512:(j + 1) * 512], op=ALU.add),
             [Bp, Bb], [Bo])
    P.dma("sp", out, ot[:], [Bo], [Bout])
    cx.finish()


def stream_linear(cx, W, col0, n_m, n_kc, act, TL, wring, psring, epilogue, row0=0, CW=256):
    P = cx.P
    ntb = TL // 512
    mpw = CW // 128
    for wt_i in range(n_m // mpw):
        wt, Bw = wring.next()
        c0 = col0 + wt_i * CW
        src = W[row0:row0 + n_kc * 128, c0:c0 + CW].rearrange("(kc p) j -> p kc j", p=128)
        P.dma("pool", wt[:, 0:n_kc, 0:CW], src, [], [Bw])
        for mi in range(mpw):
            m = wt_i * mpw + mi
            ps, Bp = psring.next()
            for tb in range(ntb):
                for kc in range(n_kc):
                    a_ap, a_bufs = act(kc, tb * 512, (tb + 1) * 512)
                    P.mm(ps[:, tb * 512:(tb + 1) * 512], wt[:, kc, mi * 128:(mi + 1) * 128],
                         a_ap, kc == 0, kc == n_kc - 1, [Bw] + a_bufs, [Bp])
            epilogue(m, ps, Bp)


def load_mod(cx, modb, adat, idx_list):
    P = cx.P
    a = cx.sb([128, 6, KC], F32, "moda")
    b = cx.sb([128, 6, KC], F32, "modb")
    m = cx.sb([128, 6, KC], F32, "mod")
    Ba, Bb, Bm = cx.buf("moda"), cx.buf("modb"), cx.buf("mod")
    P.dma("sp", a[:], modb, [], [Ba])
    P.dma("sp", b[:], adat, [], [Bb])
    P.op("dve", lambda e: e.tensor_tensor(out=m[:], in0=a[:], in1=b[:], op=ALU.add), [Ba, Bb], [Bm])
    for i in idx_list:
        P.op("dve", lambda e, i=i: e.tensor_scalar_add(out=m[:, i, :], in0=m[:, i, :], scalar1=1.0), [Bm], [Bm])
    return m, Bm


def phase_p1(nc, xT, modb, adat, w_in, uT_out, gates_out, TL, gcol0=OFF_GA):
    cx = Ctx(nc)
    P = cx.P
    mod, Bm = load_mod(cx, modb, adat, [1])
    uT = cx.sb([128, KC, TL], BF16, "uT")
    BuT = [cx.buf(f"uT{k}") for k in range(KC)]
    Buo = cx.buf("uTout")
    xr = sb_ring(cx, 3, [128, TL], F32, "x")
    for kc in range(KC):
        xt, Bx = xr.next()
        P.dma("sp", xt[:], xT[kc], [], [Bx])
        eng = "dve" if kc % 2 == 0 else "act"
        if eng == "dve":
            P.op("dve", lambda e, xt=xt, kc=kc: e.tensor_scalar(
                out=uT[:, kc, :], in0=xt[:], scalar1=mod[:, 1, kc:kc + 1], scalar2=mod[:, 0, kc:kc + 1],
                op0=ALU.mult, op1=ALU.add), [Bx, Bm], [BuT[kc]])
        else:
            P.op("act", lambda e, xt=xt, kc=kc: e.activation(
                out=uT[:, kc, :], in_=xt[:], func=AF.Identity, bias=mod[:, 0, kc:kc + 1],
                scale=mod[:, 1, kc:kc + 1]), [Bx, Bm], [BuT[kc]])
    P.dma("sp", uT_out.rearrange("kc p t -> p kc t"), uT[:], BuT, [Buo])
    wring = sb_ring(cx, 3, [128, KC, 256], BF16, "w")
    psring = ps_ring(cx, 4, [128, TL], "ps")
    gr = sb_ring(cx, 3, [128, TL], BF16, "g")
    gor = Ring([cx.buf(f"go{i}") for i in range(4)])

    def epi(m, ps, Bp):
        gt, Bg = gr.next()
        P.op("act", lambda e: e.activation(out=gt[:], in_=ps[:], func=AF.Sigmoid), [Bp], [Bg])
        P.dma("sp", gates_out[m], gt[:], [Bg], [gor.next()])

    stream_linear(cx, w_in, gcol0, 64, KC, lambda kc, a, b: (uT[:, kc, a:b], [BuT[kc]]), TL, wring, psring, epi)
    cx.finish()


NFM = 1088
NTM = 324
NWC = NFM + NTM


def phase_p2a(nc, UT_all, Wc, QKVT, QBT, TMZ, T):
    cx = Ctx(nc)
    P = cx.P
    wfm = cx.sb([128, KC, NFM], BF16, "wfm")
    wtm = cx.sb([128, KC, NTM], BF16, "wtm")
    Bw = cx.buf("w")

    def wsrc(c0, n):
        return Wc[:, c0:c0 + n].rearrange("(kc p) j -> p kc j", p=128)
    for c0 in range(0, NFM, 128):
        n = min(128, NFM - c0)
        P.dma("pool", wfm[:, :, c0:c0 + n], wsrc(c0, n), [], [Bw])
    for c0 in range(0, NTM, 128):
        n = min(128, NTM - c0)
        P.dma("pool", wtm[:, :, c0:c0 + n], wsrc(NFM + c0, n), [], [Bw])
    ur = sb_ring(cx, 2, [128, KC, 512], BF16, "ut")
    psr = ps_ring(cx, 3, [128, 512], "psf")
    pst = ps_ring(cx, 2, [128, NTM], "pst")
    fo = sb_ring(cx, 3, [128, 512], F32, "fo")
    to = sb_ring(cx, 3, [128, NTM], F32, "to")
    dor = Ring([cx.buf(f"do{i}") for i in range(4)])
    groups = [(g * 128, 128) for g in range(6)] + [(768 + 64 * g, 64) for g in range(5)]
    for tb in range(T // 512):
        ut, But = ur.next()
        P.dma("sp", ut[:], UT_all[:, :, tb * 512:(tb + 1) * 512].rearrange("kc p t -> p kc t"), [], [But])
        for g, (c0, m) in enumerate(groups):
            ps, Bp = psr.next()
            for kc in range(KC):
                P.mm(ps[0:m, :], wfm[:, kc, c0:c0 + m], ut[:, kc, :], kc == 0, kc == KC - 1, [Bw, But], [Bp])
            ft, Bf = fo.next()
            if g % 2 == 0:
                P.op("act", lambda e, ft=ft, ps=ps, m=m: e.copy(out=ft[0:m, :], in_=ps[0:m, :]), [Bp], [Bf])
            else:
                P.op("act", lambda e, ft=ft, ps=ps, m=m: e.copy(out=ft[0:m, :], in_=ps[0:m, :]), [Bp], [Bf])
            dst = QKVT[g, :, tb * 512:(tb + 1) * 512] if g < 6 else QBT[g - 6, :, tb * 512:(tb + 1) * 512]
            P.dma("sp", dst, ft[0:m, :], [Bf], [dor.next()])
        for tt in range(4):
            ps, Bp = pst.next()
            for kc in range(KC):
                P.mm(ps[:], ut[:, kc, tt * 128:(tt + 1) * 128], wtm[:, kc, :], kc == 0, kc == KC - 1, [But, Bw], [Bp])
            tt_, Bt = to.next()
            P.op("act", lambda e, tt_=tt_, ps=ps: e.copy(out=tt_[:], in_=ps[:]), [Bp], [Bt])
            j = tb * 4 + tt
            P.dma("sp", TMZ[j * 128:(j + 1) * 128, :], tt_[:], [Bt], [dor.next()])
    cx.finish()


def phase_p2c(nc, QBT, TMZ, biasT, maskc, sinkb, ident, oB, T):
    cx = Ctx(nc)
    P = cx.P
    NT = T // 128
    qk = cx.sb([64, 5, T], F32, "qk")
    V = cx.sb([128, NT + 1, 64], F32, "V")
    bm = cx.sb([128, 4, 256], F32, "bm")
    bm0 = cx.sb([128, 4, 256], F32, "bm0")
    mk = cx.sb([128, 2, 256], F32, "mk")
    sk = cx.sb([128, 4], F32, "sk")
    idt = cx.sb([128, 128], F32, "id")
    Bq, Bv, Bc, Bbm, Bbm0 = [cx.buf(n) for n in "q v c bm bm0".split()]
    for g in range(5):
        P.dma("sp", qk[:, g, :], QBT[g], [], [Bq])
    P.op("pool", lambda e: e.memset(V[:, 0, :], 0.0), [], [Bv])
    P.dma("sp", V[:, 1:NT + 1, :], TMZ[:, 256:320].rearrange("(j p) c -> p j c", p=128), [], [Bv])
    P.dma("sp", bm[:], biasT.rearrange("h p k -> p h k"), [], [Bc])
    P.dma("sp", mk[:], maskc.rearrange("h p k -> p h k"), [], [Bc])
    P.dma("sp", sk[:], sinkb, [], [Bc])
    P.dma("sp", idt[:], ident, [], [Bc])
    for j in range(4):
        P.op("dve", lambda e, j=j: e.tensor_tensor(out=bm0[:, j, :], in0=bm[:, j, :], in1=mk[:, 1, :], op=ALU.add),
             [Bc], [Bbm0])
    bmm = cx.sb([128, 4, 256], F32, "bmm")
    for j in range(4):
        P.op("dve", lambda e, j=j: e.tensor_tensor(out=bmm[:, j, :], in0=bm[:, j, :], in1=mk[:, 0, :], op=ALU.add),
             [Bc], [Bbm])
    pss = ps_ring(cx, 2, [128, 256], "pss")
    pst = ps_ring(cx, 2, [128, 256], "pst")
    pso = ps_ring(cx, 2, [128, 64], "pso")
    sr = sb_ring(cx, 3, [128, 256], F32, "s")
    pr = sb_ring(cx, 3, [128, 256], F32, "p")
    ptr = sb_ring(cx, 3, [128, 256], F32, "pt")
    st = sb_ring(cx, 4, [128, 8], F32, "st")
    osb = sb_ring(cx, 2, [128, 256], F32, "osb")
    dor = Ring([cx.buf(f"do{i}") for i in range(2)])
    for n in range(NT):
        ot, Bo = osb.next()
        for j in range(4):
            ps, Bp = pss.next()
            if n == 0:
                P.mm(ps[:, 128:256], qk[:, j, 0:128], qk[:, 4, 0:128], True, True, [Bq], [Bp])
                P.mm(ps[:, 0:128], qk[:, j, 0:128], qk[:, 4, 0:128], True, True, [Bq], [Bp])
            else:
                P.mm(ps[:], qk[:, j, n * 128:(n + 1) * 128], qk[:, 4, (n - 1) * 128:(n + 1) * 128], True, True, [Bq], [Bp])
            s, Bs = sr.next()
            bsrc = bm0 if n == 0 else bmm
            P.op("dve", lambda e, s=s, ps=ps, bsrc=bsrc, j=j: e.scalar_tensor_tensor(
                out=s[:], in0=ps[:], scalar=0.125, in1=bsrc[:, j, :], op0=ALU.mult, op1=ALU.add),
                [Bp, Bbm, Bbm0], [Bs])
            sv, Bsv = st.next()
            P.op("dve", lambda e, s=s, sv=sv: e.reduce_max(out=sv[:, 0:1], in_=s[:], axis=AX.X), [Bs], [Bsv])
            P.op("dve", lambda e, sv=sv, j=j: e.tensor_scalar(out=sv[:, 1:2], in0=sv[:, 0:1], scalar1=sk[:, j:j + 1],
                                                           scalar2=-1.0, op0=ALU.max, op1=ALU.mult), [Bsv, Bc], [Bsv])
            p, Bpp = pr.next()
            P.op("act", lambda e, p=p, s=s, sv=sv: e.activation(out=p[:], in_=s[:], func=AF.Exp, bias=sv[:, 1:2],
                                                              scale=1.0), [Bs, Bsv], [Bpp])
            P.op("dve", lambda e, p=p, sv=sv: e.reduce_sum(out=sv[:, 2:3], in_=p[:], axis=AX.X), [Bpp, Bsv], [Bsv])
            P.op("act", lambda e, sv=sv, j=j: e.activation(out=sv[:, 3:4], in_=sk[:, j:j + 1], func=AF.Exp,
                                                         bias=sv[:, 1:2], scale=1.0), [Bsv, Bc], [Bsv])
            P.op("dve", lambda e, sv=sv: e.tensor_tensor(out=sv[:, 4:5], in0=sv[:, 2:3], in1=sv[:, 3:4], op=ALU.add),
                 [Bsv], [Bsv])
            P.op("dve", lambda e, sv=sv: e.reciprocal(out=sv[:, 5:6], in_=sv[:, 4:5]), [Bsv], [Bsv])
            pt_ps, Bptp = pst.next()
            for hf in range(2):
                P.mm(pt_ps[:, hf * 128:(hf + 1) * 128], p[:, hf * 128:(hf + 1) * 128], idt[:], True, True, [Bpp, Bc], [Bptp])
            pt, Bpt = ptr.next()
            P.op("act", lambda e, pt=pt, pt_ps=pt_ps: e.copy(out=pt[:], in_=pt_ps[:]), [Bptp], [Bpt])
            po, Bpo = pso.next()
            for hf in range(2):
                P.mm(po[:], pt[:, hf * 128:(hf + 1) * 128], V[:, n + hf, :], hf == 0, hf == 1, [Bpt, Bv], [Bpo])
            P.op("dve", lambda e, ot=ot, po=po, sv=sv, j=j: e.tensor_scalar(
                out=ot[:, j * 64:(j + 1) * 64], in0=po[:], scalar1=sv[:, 5:6], scalar2=None, op0=ALU.mult),
                [Bpo, Bsv], [Bo])
        P.dma("sp", oB[n * 128:(n + 1) * 128, :], ot[:], [Bo], [dor.next()])
    cx.finish()


def phase_p2b(nc, QKVT, TMZ, convw, hc, normrep, cst, oA, T):
    STAGE = 9
    cx = Ctx(nc)
    P = cx.P
    NCH = T // 64
    NB = T // 512
    Bc = cx.buf("c")
    cw = cx.sb([128, 6, 4], F32, "cw")
    hcs = cx.sb([64, 4], F32, "hc")
    nrep = cx.sb([64, 8, 256], F32, "nrep")
    cs_ = cx.sb([128, 5, 128], F32, "cst")
    P.dma("sp", cw[:], convw, [], [Bc])
    P.dma("sp", hcs[:], hc, [], [Bc])
    P.dma("sp", nrep[:], normrep, [], [Bc])
    P.dma("sp", cs_[:], cst, [], [Bc])
    ident = cs_[:, 0, :]
    ones = cs_[:, 1, :]
    tri2 = cs_[0:64, 2, :]
    mask2 = cs_[0:64, 3, :]
    strict = cs_[0:64, 4, 0:64]
    id64 = cs_[0:64, 0, 0:64]
    ab = cx.sb([64, NCH, 4], F32, "ab")
    Bab = cx.buf("ab")
    for c0 in range(0, NCH, 16):
        c1 = min(NCH, c0 + 16)
        P.dma("sp", ab[:, c0:c1, :], TMZ[c0 * 64:c1 * 64, 320:324].rearrange("(n p) c -> p n c", p=64), [], [Bab])
    beta = cx.sb([64, NCH, 2], F32, "beta")
    gg = cx.sb([64, NCH, 2], F32, "gg")
    x1 = cx.sb([64, NCH, 2], F32, "x1")
    negA = cx.sb([64, 2], F32, "negA")
    Gc = cx.sb([64, NCH, 2], F32, "Gc")
    expG = cx.sb([64, NCH, 2], F32, "expG")
    edec = cx.sb([64, NCH, 2], F32, "edec")
    bexp = cx.sb([64, NCH, 2], F32, "bexp")
    eGl = cx.sb([128, NCH, 2], F32, "eGl")
    Bpre = cx.buf("pre")
    P.op("act", lambda e: e.activation(out=beta[:], in_=ab[:, :, 0:2], func=AF.Sigmoid), [Bab], [Bpre])
    P.op("act", lambda e: e.activation(out=negA[:], in_=hcs[:, 0:2], func=AF.Exp), [Bc], [Bpre])
    P.op("dve", lambda e: e.tensor_scalar(out=negA[:], in0=negA[:], scalar1=-1.0, scalar2=None, op0=ALU.mult), [Bpre], [Bpre])
    for h in range(2):
        P.op("dve", lambda e, h=h: e.tensor_scalar(out=x1[:, :, h], in0=ab[:, :, 2 + h], scalar1=hcs[:, 2 + h:3 + h],
                                                 scalar2=None, op0=ALU.add), [Bab, Bc], [Bpre])
    P.op("act", lambda e: e.activation(out=x1[:], in_=x1[:], func=AF.Exp), [Bpre], [Bpre])
    P.op("dve", lambda e: e.tensor_scalar(out=x1[:], in0=x1[:], scalar1=1.0, scalar2=None, op0=ALU.add), [Bpre], [Bpre])
    P.op("act", lambda e: e.activation(out=x1[:], in_=x1[:], func=AF.Ln), [Bpre], [Bpre])
    for h in range(2):
        P.op("dve", lambda e, h=h: e.tensor_scalar(out=gg[:, :, h], in0=x1[:, :, h], scalar1=negA[:, h:h + 1],
                                                 scalar2=None, op0=ALU.mult), [Bpre], [Bpre])
    banks = [(cx.ps([128, 512], F32, f"bk{i}")) for i in range(6)]
    Bbank = [cx.buf(f"bank{i}") for i in range(6)]
    Bb0, Bb1 = Bbank[0], Bbank[1]
    ggf = gg[:].rearrange("p n h -> p (n h)")
    P.mm(banks[0][0:64, 0:NCH * 2], tri2[:, 0:64], ggf, True, True, [Bc, Bpre], [Bb0])
    P.mm(banks[1][:, 0:NCH * 2], ones[0:64, :], ggf, True, True, [Bc, Bpre], [Bb1])
    Gcf = Gc[:].rearrange("p n h -> p (n h)")
    P.op("act", lambda e: e.copy(out=Gcf, in_=banks[0][0:64, 0:NCH * 2]), [Bb0], [Bpre])
    P.op("act", lambda e: e.activation(out=expG[:].rearrange("p n h -> p (n h)"), in_=Gcf, func=AF.Exp), [Bpre], [Bpre])
    P.op("dve", lambda e: e.tensor_tensor(out=edec[:].rearrange("p n h -> p (n h)"), in0=banks[1][0:64, 0:NCH * 2], in1=Gcf,
                                        op=ALU.subtract), [Bb1, Bpre], [Bpre])
    P.op("act", lambda e: e.activation(out=edec[:], in_=edec[:], func=AF.Exp), [Bpre], [Bpre])
    P.op("act", lambda e: e.activation(out=eGl[:].rearrange("p n h -> p (n h)"), in_=banks[1][:, 0:NCH * 2], func=AF.Exp),
         [Bb1], [Bpre])
    P.op("dve", lambda e: e.tensor_tensor(out=bexp[:], in0=beta[:], in1=expG[:], op=ALU.mult), [Bpre], [Bpre])
    S = [[cx.sb([128, 128], F32, f"S{h}{i}") for i in range(2)] for h in range(2)]
    BS = [[cx.buf(f"S{h}{i}") for i in range(2)] for h in range(2)]
    for h in range(2):
        P.op("pool", lambda e, h=h: e.memset(S[h][0][:], 0.0), [], [BS[h][0]])
    scur = [0, 0]
    rawr = sb_ring(cx, 2, [128, 6, 515], F32, "raw")
    accr = sb_ring(cx, 2, [128, 512], F32, "acc")
    csr = sb_ring(cx, 2, [128, 6, 512], F32, "cs")
    sqr = sb_ring(cx, 2, [128, 512], F32, "sq")
    rnr = sb_ring(cx, 2, [128, 512], F32, "rn")
    qknr = sb_ring(cx, 2, [128, 4, 512], F32, "qkn")
    ztr = sb_ring(cx, 2, [64, 8, 256], F32, "zt")
    obr = sb_ring(cx, 2, [64, 16, 128], F32, "ob")
    osq = cx.sb([64, 16, 128], F32, "osq")
    Bosq = cx.buf("osq")
    nst = sb_ring(cx, 2, [64, 32], F32, "nst")
    ogr = sb_ring(cx, 2, [64, 8, 256], F32, "og")
    dor = Ring([cx.buf(f"do{i}") for i in range(2)])

    def psr(bank, col0, w, rows):
        return Ring([(banks[bank][0:rows, col0:col0 + w], Bbank[bank])])
    r_tk = psr(1, 0, 256, 64)
    r_gr = psr(2, 0, 128, 64)
    r_kq = psr(2, 256, 128, 64)
    r_tt = psr(3, 0, 128, 64)
    r_sq = psr(4, 0, 128, 64)
    r_pq = psr(5, 0, 128, 64)
    r_wt = psr(3, 128, 64, 128)
    r_u = psr(3, 256, 128, 64)
    r_ws = psr(4, 256, 256, 64)
    r_iv = psr(5, 256, 128, 64)
    r_kv = psr(1, 256, 128, 128)
    Bssq = Bbank[0]

    def sring(n, shape, name):
        return sb_ring(cx, n, shape, F32, name)
    s_kbg, s_kdec, s_vb = sring(3, [128, 128], "kbg"), sring(3, [128, 128], "kdec"), sring(3, [64, 128], "vb")
    s_gbt, s_eD, s_Dm, s_X = sring(2, [64, 64], "gbt"), sring(2, [64, 128], "eD"), sring(2, [64, 128], "Dm"), sring(2, [64, 128], "X")
    s_UL = sring(4, [64, 128], "UL")
    s_PQ = sring(4, [128, 128], "PQ")
    s_iT = sring(3, [64, 64], "iT")
    s_wT, s_u = sring(3, [128, 64], "wT"), sring(3, [64, 128], "u")
    s_vn, s_oq = sring(3, [128, 128], "vn"), sring(3, [64, 128], "oq")
    for rg in (s_kbg, s_kdec, s_PQ, s_vn):
        for (tl, Bt_) in rg.items:
            P.op("pool", lambda e, tl=tl: e.memset(tl[64:128, :], 0.0), [], [Bt_])
    I2 = cs_[0:64, 4, :]
    i2t = cx.sb([64, 128], F32, "I2")
    BI2 = cx.buf("I2")
    P.op("pool", lambda e: e.tensor_copy(out=i2t[:, 0:64], in_=id64), [Bc], [BI2])
    P.op("pool", lambda e: e.tensor_copy(out=i2t[:, 64:128], in_=id64), [Bc], [BI2])

    for b in range(NB if STAGE >= 2 else 0):
        raw, Braw = rawr.next()
        if b == 0:
            P.op("pool", lambda e, raw=raw: e.memset(raw[:, :, 0:3], 0.0), [], [Braw])
            P.dma("sp", raw[:, :, 3:515], QKVT[:, :, 0:512].rearrange("g p t -> p g t"), [], [Braw])
        else:
            P.dma("sp", raw[:], QKVT[:, :, b * 512 - 3:(b + 1) * 512].rearrange("g p t -> p g t"), [], [Braw])
        zt, Bz = ztr.next()
        P.dma("sp", zt[:], TMZ[b * 512:(b + 1) * 512, 0:256].rearrange("(n p) c -> p n c", p=64), [], [Bz])
        P.op("act", lambda e, zt=zt: e.activation(out=zt[:], in_=zt[:], func=AF.Silu), [Bz], [Bz])
        P.op("pool", lambda e, zt=zt: e.tensor_tensor(out=zt[:], in0=zt[:], in1=nrep[:], op=ALU.mult), [Bz, Bc], [Bz])
        cs, Bcs = csr.next()
        qkn, Bqkn = qknr.next()
        for g in range(6):
            acc, Bacc = accr.next()
            eng = "dve"
            P.op("pool", lambda e, acc=acc, raw=raw, g=g: e.tensor_scalar(out=acc[:], in0=raw[:, g, 3:515], scalar1=cw[:, g, 3:4],
                                                                   scalar2=None, op0=ALU.mult), [Braw, Bc], [Bacc])
            for j in range(3):
                P.op(eng, lambda e, acc=acc, raw=raw, g=g, j=j: e.scalar_tensor_tensor(
                    out=acc[:], in0=raw[:, g, j:j + 512], scalar=cw[:, g, j:j + 1], in1=acc[:], op0=ALU.mult, op1=ALU.add),
                    [Braw, Bc, Bacc], [Bacc])
            P.op("act", lambda e, acc=acc, cs=cs, g=g: e.activation(out=cs[:, g, :], in_=acc[:], func=AF.Silu), [Bacc], [Bcs])
            if g < 4:
                sq, Bsq = sqr.next()
                P.op("pool", lambda e, sq=sq, cs=cs, g=g: e.tensor_tensor(out=sq[:], in0=cs[:, g, :], in1=cs[:, g, :], op=ALU.mult),
                     [Bcs], [Bsq])
                P.mm(banks[0][:, :], ones, sq[:], True, True, [Bc, Bsq], [Bssq])
                rn, Brn = rnr.next()
                P.op("dve", lambda e, rn=rn: e.tensor_scalar(out=rn[:], in0=banks[0][:, :], scalar1=NORM_EPS, scalar2=None,
                                                          op0=ALU.add), [Bssq], [Brn])
                P.op("act", lambda e, rn=rn: e.activation(out=rn[:], in_=rn[:], func=AF.Sqrt), [Brn], [Brn])
                P.op("dve", lambda e, rn=rn: e.reciprocal(out=rn[:], in_=rn[:]), [Brn], [Brn])
                sc = (128.0 ** -0.5) if g < 2 else 1.0
                P.op("dve", lambda e, rn=rn, cs=cs, qkn=qkn, g=g, sc=sc: e.scalar_tensor_tensor(
                    out=qkn[:, g, :], in0=cs[:, g, :], scalar=sc, in1=rn[:], op0=ALU.mult, op1=ALU.mult), [Bcs, Brn], [Bqkn])
        ob, Bob = obr.next()
        if STAGE < 3:
            continue
        for c in range(8):
            n = b * 8 + c
            sl = slice(c * 64, (c + 1) * 64)
            for h in range(2):
                qT = qkn[:, h, sl]
                kT = qkn[:, 2 + h, sl]
                vT = cs[:, 4 + h, sl]
                col = lambda t, n=n, h=h: t[:, n, h:h + 1]
                tk, Btk = r_tk.next()
                P.mm(tk[:, 0:128], kT, ident, True, True, [Bqkn, Bc], [Btk])
                P.mm(tk[:, 128:256], vT, ident, True, True, [Bcs, Bc], [Btk])
                kbg, Bkbg = s_kbg.next()
                kdec, Bkdec = s_kdec.next()
                vb, Bvb = s_vb.next()
                P.op("dve", lambda e, kbg=kbg, tk=tk, col=col: e.tensor_scalar(out=kbg[0:64, :], in0=tk[:, 0:128], scalar1=col(bexp),
                                                                            scalar2=None, op0=ALU.mult), [Btk, Bpre], [Bkbg])
                P.op("dve", lambda e, kdec=kdec, tk=tk, col=col: e.tensor_scalar(out=kdec[0:64, :], in0=tk[:, 0:128], scalar1=col(edec),
                                                                              scalar2=None, op0=ALU.mult), [Btk, Bpre], [Bkdec])
                P.op("dve", lambda e, vb=vb, tk=tk, col=col: e.tensor_scalar(out=vb[:], in0=tk[:, 128:256], scalar1=col(beta),
                                                                          scalar2=None, op0=ALU.mult), [Btk, Bpre], [Bvb])
                gbt, Bgbt = s_gbt.next()
                P.op("pool", lambda e, gbt=gbt, col=col: e.tensor_scalar(out=gbt[:], in0=ones[0:64, 0:64], scalar1=col(gg),
                                                                      scalar2=None, op0=ALU.mult), [Bc, Bpre], [Bgbt])
                gr, Bgr = r_gr.next()
                P.mm(gr, gbt[:], tri2, True, True, [Bgbt, Bc], [Bgr])
                kq, Bkq = r_kq.next()
                P.mm(kq[:, 0:64], kT, kT, True, True, [Bqkn], [Bkq])
                P.mm(kq[:, 64:128], qT, kT, True, True, [Bqkn], [Bkq])
                eD, BeD = s_eD.next()
                P.op("act", lambda e, eD=eD, gr=gr, col=col: e.activation(out=eD[:], in_=gr, func=AF.Exp, bias=col(Gc), scale=-1.0),
                     [Bgr, Bpre], [BeD])
                Dm, BDm = s_Dm.next()
                P.op("dve", lambda e, Dm=Dm, eD=eD: e.scalar_tensor_tensor(out=Dm[:], in0=eD[:], scalar=1.0, in1=mask2,
                                                                        op0=ALU.min, op1=ALU.mult), [BeD, Bc], [BDm])
                X, BX = s_X.next()
                P.op("dve", lambda e, X=X, kq=kq, Dm=Dm: e.tensor_tensor(out=X[:], in0=kq, in1=Dm[:], op=ALU.mult), [Bkq, BDm], [BX])
                UL, BUL = s_UL.next()
                P.op("dve", lambda e, UL=UL, X=X, col=col: e.scalar_tensor_tensor(
                    out=UL[:, 64:128], in0=X[:, 0:64], scalar=col(beta), in1=strict, op0=ALU.mult, op1=ALU.mult),
                    [BX, Bpre, Bc], [BUL])
                tt, Btt = r_tt.next()
                P.mm(tt[:, 0:64], UL[:, 64:128], id64, True, True, [BUL, Bc], [Btt])
                P.mm(tt[:, 64:128], X[:, 64:128], id64, True, True, [BX, Bc], [Btt])
                iT, BiT = s_iT.next()
                P.op("act", lambda e, UL=UL, tt=tt: e.copy(out=UL[:, 0:64], in_=tt[:, 0:64]), [Btt], [BUL])
                P.op("act", lambda e, iT=iT, tt=tt: e.copy(out=iT[:], in_=tt[:, 64:128]), [Btt], [BiT])
                PQ, BPQ = s_PQ.next()
                P.op("pool", lambda e, PQ=PQ, UL=UL: e.tensor_tensor(out=PQ[0:64, :], in0=i2t[:], in1=UL[:], op=ALU.subtract),
                     [BI2, BUL], [BPQ])
                for k in range(1, 6):
                    last = (k == 5)
                    sqp, Bsqp = r_sq.next()
                    P.mm(sqp[:, 0:64], UL[:, 64:128], UL[:, 0:64], True, True, [BUL], [Bsqp])
                    if not last:
                        P.mm(sqp[:, 64:128], UL[:, 0:64], UL[:, 64:128], True, True, [BUL], [Bsqp])
                    UL2, BUL2 = s_UL.next()
                    w_ = 64 if last else 128
                    P.op("act", lambda e, UL2=UL2, sqp=sqp, w_=w_: e.copy(out=UL2[:, 0:w_], in_=sqp[:, 0:w_]), [Bsqp], [BUL2])
                    pq, Bpq = r_pq.next()
                    P.mm(pq[:, 0:64], PQ[0:64, 64:128], UL2[:, 0:64], True, True, [BPQ, BUL2], [Bpq])
                    if not last:
                        P.mm(pq[:, 64:128], PQ[0:64, 0:64], UL2[:, 64:128], True, True, [BPQ, BUL2], [Bpq])
                    PQ2, BPQ2 = s_PQ.next()
                    P.op("dve", lambda e, PQ2=PQ2, PQ=PQ, pq=pq, w_=w_: e.tensor_tensor(out=PQ2[0:64, 0:w_], in0=PQ[0:64, 0:w_], in1=pq[:, 0:w_],
                                                                                     op=ALU.add), [BPQ, Bpq], [BPQ2])
                    UL, BUL, PQ, BPQ = UL2, BUL2, PQ2, BPQ2
                if STAGE < 4:
                    continue
                AiT = PQ[0:64, 0:64]
                wt, Bwt = r_wt.next()
                P.mm(wt, kbg[:], PQ[:, 0:64], True, True, [Bkbg, BPQ], [Bwt])
                if STAGE == 410:
                    continue
                wT, BwT = s_wT.next()
                P.op("act", lambda e, wT=wT, wt=wt: e.copy(out=wT[:], in_=wt), [Bwt], [BwT])
                if STAGE == 411:
                    continue
                up, Bup = r_u.next()
                P.mm(up, AiT, vb[:], True, True, [BPQ, Bvb], [Bup])
                if STAGE == 412:
                    continue
                u_, Bu_ = s_u.next()
                P.op("act", lambda e, u_=u_, up=up: e.copy(out=u_[:], in_=up), [Bup], [Bu_])
                if STAGE == 41:
                    continue
                Sc, BSc = S[h][scur[h]], BS[h][scur[h]]
                Sn, BSn = S[h][1 - scur[h]], BS[h][1 - scur[h]]
                ws, Bws = r_ws.next()
                P.mm(ws[:, 0:128], wT[:], Sc[:], True, True, [BwT, BSc], [Bws])
                P.mm(ws[:, 128:256], qT, Sc[:], True, True, [Bqkn, BSc], [Bws])
                vn, Bvn = s_vn.next()
                P.op("dve", lambda e, vn=vn, u_=u_, ws=ws: e.tensor_tensor(out=vn[0:64, :], in0=u_[:], in1=ws[:, 0:128], op=ALU.subtract),
                     [Bu_, Bws], [Bvn])
                oq, Boq = s_oq.next()
                P.op("dve", lambda e, oq=oq, ws=ws, col=col: e.tensor_scalar(out=oq[:], in0=ws[:, 128:256], scalar1=col(expG),
                                                                          scalar2=None, op0=ALU.mult), [Bws, Bpre], [Boq])
                if STAGE == 42:
                    continue
                iv, Biv = r_iv.next()
                P.mm(iv, iT[:], vn[0:64, :], True, True, [BiT, Bvn], [Biv])
                kv, Bkv = r_kv.next()
                P.mm(kv, kdec[:], vn[:], True, True, [Bkdec, Bvn], [Bkv])
                P.op("dve", lambda e, ob=ob, oq=oq, iv=iv, c=c, h=h: e.tensor_tensor(out=ob[:, c * 2 + h, :], in0=oq[:], in1=iv, op=ALU.add),
                     [Boq, Biv], [Bob])
                if STAGE == 43:
                    scur[h] = 1 - scur[h]
                    continue
                P.op("pool", lambda e, Sn=Sn, Sc=Sc, n=n, h=h: e.tensor_scalar(out=Sn[:], in0=Sc[:], scalar1=eGl[:, n, h:h + 1], scalar2=None,
                                                                            op0=ALU.mult), [BSc, Bpre], [BSn])
                P.op("dve", lambda e, Sn=Sn, kv=kv: e.tensor_tensor(out=Sn[:], in0=kv, in1=Sn[:], op=ALU.add), [Bkv, BSn], [BSn])
                scur[h] = 1 - scur[h]
        if STAGE < 5:
            continue
        P.op("pool", lambda e, ob=ob: e.tensor_tensor(out=osq[:], in0=ob[:], in1=ob[:], op=ALU.mult), [Bob], [Bosq])
        ns, Bns = nst.next()
        P.op("dve", lambda e, ns=ns: e.reduce_sum(out=ns[:, 0:16], in_=osq[:], axis=AX.X), [Bosq], [Bns])
        P.op("dve", lambda e, ns=ns: e.tensor_scalar(out=ns[:, 16:32], in0=ns[:, 0:16], scalar1=1.0 / 128.0, scalar2=NORM_EPS,
                                                  op0=ALU.mult, op1=ALU.add), [Bns], [Bns])
        P.op("act", lambda e, ns=ns: e.activation(out=ns[:, 16:32], in_=ns[:, 16:32], func=AF.Sqrt), [Bns], [Bns])
        P.op("dve", lambda e, ns=ns: e.reciprocal(out=ns[:, 16:32], in_=ns[:, 16:32]), [Bns], [Bns])
        og, Bog = ogr.next()
        for c in range(8):
            for h in range(2):
                P.op("dve", lambda e, og=og, ob=ob, ns=ns, zt=zt, c=c, h=h: e.scalar_tensor_tensor(
                    out=og[:, c, h * 128:(h + 1) * 128], in0=ob[:, c * 2 + h, :], scalar=ns[:, 16 + c * 2 + h:17 + c * 2 + h],
                    in1=zt[:, c, h * 128:(h + 1) * 128], op0=ALU.mult, op1=ALU.mult), [Bob, Bns, Bz], [Bog])
        P.dma("sp", oA[b * 512:(b + 1) * 512, :].rearrange("(n p) c -> p n c", p=64), og[:], [Bog], [dor.next()])
    cx.finish()


def k1_consts():
    c = np.zeros((128, 5, 128), np.float32)
    c[:, 0, :] = np.eye(128)
    c[:, 1, :] = 1.0
    p = np.arange(64)[:, None]; i = np.arange(64)[None, :]
    tri_le = (p <= i).astype(np.float32)
    c[0:64, 2, 0:64] = tri_le; c[0:64, 2, 64:128] = tri_le
    tril = (i <= p).astype(np.float32)
    c[0:64, 3, 0:64] = tril; c[0:64, 3, 64:128] = tril
    c[0:64, 4, 0:64] = (i < p).astype(np.float32)
    c[0:64, 4, 64:128] = np.eye(64)
    return c


def phase_p3(nc, oAT, oBT, gates, w_up_a, w_up_b, w_o, xT, modb, adat, rT, t0, TB=512):
    cx = Ctx(nc)
    P = cx.P
    mod, Bm = load_mod(cx, modb, adat, [])
    ts = slice(t0, t0 + TB)
    oa = cx.sb([128, 16, TB], BF16, "oa")
    ob = cx.sb([128, 16, TB], BF16, "ob")
    Boa, Bob = cx.buf("oa"), cx.buf("ob")
    P.dma("pool", oa[:], oAT[:, :, ts].rearrange("k p t -> p k t"), [], [Boa])
    P.dma("pool", ob[:], oBT[:, :, ts].rearrange("k p t -> p k t"), [], [Bob])
    mg = cx.sb([128, KC, TB], BF16, "mg")
    Bmg = [cx.buf(f"mg{m}") for m in range(KC)]
    wra = sb_ring(cx, 2, [128, 16, 256], BF16, "wa")
    wrb = sb_ring(cx, 2, [128, 16, 256], BF16, "wb")
    psr = ps_ring(cx, 4, [128, TB], "ps")
    gr = sb_ring(cx, 4, [128, TB], BF16, "g")
    t1r = sb_ring(cx, 2, [128, TB], F32, "t1")
    t2r = sb_ring(cx, 2, [128, TB], F32, "t2")
    for w2 in range(16):
        wa, Bwa = wra.next()
        wb, Bwb = wrb.next()
        P.dma("pool", wa[:], w_up_a[:, w2 * 256:(w2 + 1) * 256].rearrange("(kc p) j -> p kc j", p=128), [], [Bwa])
        P.dma("pool", wb[:], w_up_b[:, w2 * 256:(w2 + 1) * 256].rearrange("(kc p) j -> p kc j", p=128), [], [Bwb])
        for mi in range(2):
            m = w2 * 2 + mi
            ga, Bga = gr.next()
            gb, Bgb = gr.next()
            P.dma("sp", ga[:], gates[m, :, ts], [], [Bga])
            P.dma("sp", gb[:], gates[32 + m, :, ts], [], [Bgb])
            pa, Bpa = psr.next()
            for kc in range(16):
                P.mm(pa[:], wa[:, kc, mi * 128:(mi + 1) * 128], oa[:, kc, :], kc == 0, kc == 15, [Bwa, Boa], [Bpa])
            pb, Bpb = psr.next()
            for kc in range(16):
                P.mm(pb[:], wb[:, kc, mi * 128:(mi + 1) * 128], ob[:, kc, :], kc == 0, kc == 15, [Bwb, Bob], [Bpb])
            t1, Bt1 = t1r.next()
            t2, Bt2 = t2r.next()
            P.op("dve", lambda e, t1=t1, pa=pa, ga=ga: e.tensor_tensor(out=t1[:], in0=pa[:], in1=ga[:], op=ALU.mult), [Bpa, Bga], [Bt1])
            P.op("dve", lambda e, t2=t2, pb=pb, gb=gb: e.tensor_tensor(out=t2[:], in0=pb[:], in1=gb[:], op=ALU.mult), [Bpb, Bgb], [Bt2])
            P.op("pool", lambda e, t1=t1, t2=t2, m=m: e.tensor_tensor(out=mg[:, m, :], in0=t1[:], in1=t2[:], op=ALU.add), [Bt1, Bt2], [Bmg[m]])
    wring = sb_ring(cx, 2, [128, KC, 256], BF16, "wo")
    xr = sb_ring(cx, 3, [128, TB], F32, "x")
    rr = sb_ring(cx, 3, [128, TB], F32, "r")
    dor = Ring([cx.buf(f"do{i}") for i in range(3)])

    def epi(m, ps, Bp):
        xt, Bx = xr.next()
        P.dma("sp", xt[:], xT[m, :, ts], [], [Bx])
        P.op("act", lambda e, xt=xt: e.mul(out=xt[:], in_=xt[:], mul=DN_ALPHA), [Bx], [Bx])
        rt, Br = rr.next()
        P.op("dve", lambda e, rt=rt, ps=ps, xt=xt, m=m: e.scalar_tensor_tensor(
            out=rt[:], in0=ps[:], scalar=mod[:, 2, m:m + 1], in1=xt[:], op0=ALU.mult, op1=ALU.add), [Bp, Bx, Bm], [Br])
        P.dma("sp", rT[m, :, ts], rt[:], [Br], [dor.next()])
    stream_linear(cx, w_o, 0, KC, KC, lambda kc, a, b: (mg[:, kc, a:b], [Bmg[kc]]), TB, wring, psr, epi)
    cx.finish()


def phase_ln(nc, rT, lng, lnb, modb, adat, sc_idx, xoT, uoT, cstk, TL, TB=512):
    cx = Ctx(nc)
    P = cx.P
    mod, Bm = load_mod(cx, modb, adat, [sc_idx + 1])
    g = cx.sb([128, KC], F32, "lng")
    b = cx.sb([128, KC], F32, "lnb")
    cs_ = cx.sb([128, 5, 128], F32, "cst")
    Bc = cx.buf("c")
    P.dma("sp", g[:], lng, [], [Bc])
    P.dma("sp", b[:], lnb, [], [Bc])
    P.dma("sp", cs_[:], cstk, [], [Bc])
    ones = cs_[:, 1, :]
    rbuf = sb_ring(cx, 2, [128, KC, TB], F32, "r")
    sqr = sb_ring(cx, 3, [128, TB], F32, "sq")
    ps1 = ps_ring(cx, 2, [128, TB], "s1")
    ps2 = ps_ring(cx, 2, [128, TB], "s2")
    mr = sb_ring(cx, 2, [128, TB], F32, "mean")
    vr = sb_ring(cx, 2, [128, TB], F32, "var")
    tr = sb_ring(cx, 3, [128, TB], F32, "t")
    xor_ = sb_ring(cx, 3, [128, TB], F32, "xo")
    uor = sb_ring(cx, 3, [128, TB], BF16, "uo")
    dor = Ring([cx.buf(f"do{i}") for i in range(4)])
    for tb in range(TL // TB):
        ts = slice(tb * TB, (tb + 1) * TB)
        r, Br = rbuf.next()
        P.dma("sp", r[:], rT[:, :, ts].rearrange("k p t -> p k t"), [], [Br])
        s1, Bs1 = ps1.next()
        s2, Bs2 = ps2.next()
        for kc in range(KC):
            sq, Bsq = sqr.next()
            P.op("pool", lambda e, sq=sq, r=r, kc=kc: e.tensor_tensor(out=sq[:], in0=r[:, kc, :], in1=r[:, kc, :], op=ALU.mult), [Br], [Bsq])
            P.mm(s1[:], ones, r[:, kc, :], kc == 0, kc == KC - 1, [Bc, Br], [Bs1])
            P.mm(s2[:], ones, sq[:], kc == 0, kc == KC - 1, [Bc, Bsq], [Bs2])
        mean, Bmean = mr.next()
        var, Bvar = vr.next()
        P.op("dve", lambda e, mean=mean, s1=s1: e.tensor_scalar(out=mean[:], in0=s1[:], scalar1=1.0 / D, scalar2=None, op0=ALU.mult), [Bs1], [Bmean])
        P.op("dve", lambda e, var=var, s2=s2: e.tensor_scalar(out=var[:], in0=s2[:], scalar1=1.0 / D, scalar2=LN_EPS, op0=ALU.mult, op1=ALU.add), [Bs2], [Bvar])
        sq, Bsq = sqr.next()
        P.op("pool", lambda e, sq=sq, mean=mean: e.tensor_tensor(out=sq[:], in0=mean[:], in1=mean[:], op=ALU.mult), [Bmean], [Bsq])
        P.op("dve", lambda e, var=var, sq=sq: e.tensor_tensor(out=var[:], in0=var[:], in1=sq[:], op=ALU.subtract), [Bvar, Bsq], [Bvar])
        P.op("act", lambda e, var=var: e.activation(out=var[:], in_=var[:], func=AF.Sqrt), [Bvar], [Bvar])
        P.op("dve", lambda e, var=var: e.reciprocal(out=var[:], in_=var[:]), [Bvar], [Bvar])
        for kc in range(KC):
            t, Bt = tr.next()
            P.op("pool", lambda e, t=t, r=r, mean=mean, kc=kc: e.tensor_tensor(out=t[:], in0=r[:, kc, :], in1=mean[:], op=ALU.subtract), [Br, Bmean], [Bt])
            P.op("dve", lambda e, t=t, var=var: e.tensor_tensor(out=t[:], in0=t[:], in1=var[:], op=ALU.mult), [Bt, Bvar], [Bt])
            xo, Bxo = xor_.next()
            P.op("act", lambda e, xo=xo, t=t, kc=kc: e.activation(out=xo[:], in_=t[:], func=AF.Identity, bias=b[:, kc:kc + 1],
                                                                scale=g[:, kc:kc + 1]), [Bt, Bc], [Bxo])
            P.dma("sp", xoT[kc, :, ts], xo[:], [Bxo], [dor.next()])
            uo, Buo = uor.next()
            P.op("dve", lambda e, uo=uo, xo=xo, kc=kc: e.tensor_scalar(
                out=uo[:], in0=xo[:], scalar1=mod[:, sc_idx + 1, kc:kc + 1], scalar2=mod[:, sc_idx, kc:kc + 1],
                op0=ALU.mult, op1=ALU.add), [Bxo, Bm], [Buo])
            P.dma("sp", uoT[kc, :, ts], uo[:], [Buo], [dor.next()])
    cx.finish()


def phase_moe(nc, uT, xT, w_router, brep, w_gate_up, bgu, w_down, b_down, modb, adat, cstk, eye32, rT, t0, TB=256):
    cx = Ctx(nc)
    P = cx.P
    mod, Bm = load_mod(cx, modb, adat, [])
    ts = slice(t0, t0 + TB)
    NT = TB // 128
    Bc = cx.buf("c")
    cs_ = cx.sb([128, 5, 128], F32, "cst")
    ey = cx.sb([32, 32], F32, "eye")
    br = cx.sb([128, 32], F32, "brep")
    bg = cx.sb([128, NE, 4], F32, "bgu")
    wr = cx.sb([128, KC, NE], BF16, "wr")
    bd = cx.sb([32, D], BF16, "bd")
    u = cx.sb([128, KC, TB], BF16, "u")
    Bu = cx.buf("u")
    P.dma("sp", cs_[:], cstk, [], [Bc])
    P.dma("sp", ey[:], eye32, [], [Bc])
    P.dma("sp", br[:], brep, [], [Bc])
    P.dma("sp", bg[:], bgu, [], [Bc])
    Bw0 = cx.buf("w0")
    P.dma("pool", wr[:], w_router.rearrange("(kc p) e -> p kc e", p=128), [], [Bw0])
    P.dma("pool", bd[:], b_down, [], [Bw0])
    P.dma("sp", u[:], uT[:, :, ts].rearrange("k p t -> p k t"), [], [Bu])
    ident = cs_[:, 0, :]
    ones = cs_[:, 1, :]
    combT = cx.sb([32, TB], F32, "combT")
    combTb = cx.sb([32, TB], BF16, "combTb")
    BcT = cx.buf("combT")
    psl = ps_ring(cx, 2, [128, 128], "psl")
    lg = cx.sb([128, NE], F32, "lg")
    cur = cx.sb([128, NE], F32, "cur")
    eq = cx.sb([128, NE], F32, "eq")
    pe_ = cx.sb([128, NE], F32, "pe")
    mx = cx.sb([128, 8], F32, "mx")
    Brt = cx.buf("rt")
    for tt in range(NT):
        pl, Bpl = psl.next()
        for kc in range(KC):
            P.mm(pl[:, 0:NE], u[:, kc, tt * 128:(tt + 1) * 128], wr[:, kc, :], kc == 0, kc == KC - 1, [Bu, Bw0], [Bpl])
        P.op("dve", lambda e, pl=pl: e.tensor_tensor(out=lg[:], in0=pl[:, 0:NE], in1=br[:], op=ALU.add), [Bpl, Bc], [Brt])
        P.op("pool", lambda e: e.tensor_copy(out=cur[:], in_=lg[:]), [Brt], [Brt])
        for i in range(4):
            P.op("dve", lambda e, i=i: e.reduce_max(out=mx[:, i:i + 1], in_=cur[:], axis=AX.X), [Brt], [Brt])
            if i < 3:
                P.op("dve", lambda e, i=i: e.tensor_scalar(out=eq[:], in0=cur[:], scalar1=mx[:, i:i + 1], scalar2=None, op0=ALU.is_equal), [Brt], [Brt])
                P.op("dve", lambda e: e.scalar_tensor_tensor(out=cur[:], in0=eq[:], scalar=-1e30, in1=cur[:], op0=ALU.mult, op1=ALU.add), [Brt], [Brt])
        P.op("dve", lambda e: e.tensor_scalar(out=eq[:], in0=lg[:], scalar1=mx[:, 3:4], scalar2=None, op0=ALU.is_ge), [Brt], [Brt])
        P.op("dve", lambda e: e.tensor_scalar(out=mx[:, 4:5], in0=mx[:, 0:1], scalar1=-1.0, scalar2=None, op0=ALU.mult), [Brt], [Brt])
        P.op("act", lambda e: e.activation(out=pe_[:], in_=lg[:], func=AF.Exp, bias=mx[:, 4:5], scale=1.0), [Brt], [Brt])
        P.op("dve", lambda e: e.tensor_tensor(out=pe_[:], in0=pe_[:], in1=eq[:], op=ALU.mult), [Brt], [Brt])
        P.op("dve", lambda e: e.reduce_sum(out=mx[:, 5:6], in_=pe_[:], axis=AX.X), [Brt], [Brt])
        P.op("dve", lambda e: e.reciprocal(out=mx[:, 6:7], in_=mx[:, 5:6]), [Brt], [Brt])
        P.op("dve", lambda e: e.tensor_scalar(out=pe_[:], in0=pe_[:], scalar1=mx[:, 6:7], scalar2=None, op0=ALU.mult), [Brt], [Brt])
        pl2, Bpl2 = psl.next()
        P.mm(pl2[0:NE, :], pe_[:], ident, True, True, [Brt, Bc], [Bpl2])
        P.op("act", lambda e, pl2=pl2, tt=tt: e.copy(out=combT[:, tt * 128:(tt + 1) * 128], in_=pl2[0:NE, :]), [Bpl2], [BcT])
    P.op("act", lambda e: e.copy(out=combTb[:], in_=combT[:]), [BcT], [BcT])
    hall = cx.sb([128, 2 * NE, TB], BF16, "hall")
    Bh = [cx.buf(f"h{e}") for e in range(NE)]
    wgr = sb_ring(cx, 3, [128, KC, 128], BF16, "wg")
    psg = ps_ring(cx, 4, [128, TB], "psg")
    psb = ps_ring(cx, 2, [128, TB], "psb")
    cmr = sb_ring(cx, 2, [32, TB], F32, "cm")
    f32r = {n: sb_ring(cx, 2, [128, TB], F32, n) for n in ("gp", "sg", "up", "t1", "t2")}
    for ex in range(NE):
        cm, Bcm = cmr.next()
        P.op("pool", lambda e, cm=cm, ex=ex: e.tensor_scalar(out=cm[:], in0=combT[:], scalar1=ey[:, ex:ex + 1], scalar2=None, op0=ALU.mult),
             [BcT, Bc], [Bcm])
        pb, Bpb = psb.next()
        P.mm(pb[:], ones[0:32, :], cm[:], True, True, [Bc, Bcm], [Bpb])
        pss = []
        for fc in range(4):
            wg, Bwg = wgr.next()
            P.dma("pool", wg[:], w_gate_up[ex, :, fc * 128:(fc + 1) * 128].rearrange("(kc p) j -> p kc j", p=128), [], [Bwg])
            ps, Bp = psg.next()
            for kc in range(KC):
                P.mm(ps[:], wg[:, kc, :], u[:, kc, :], kc == 0, kc == KC - 1, [Bwg, Bu], [Bp])
            pss.append((ps, Bp))
        for j in range(2):
            (pg, Bpg), (pu, Bpu) = pss[j], pss[2 + j]
            gp, Bgp = f32r["gp"].next()
            sg, Bsg = f32r["sg"].next()
            up, Bup = f32r["up"].next()
            t1, Bt1 = f32r["t1"].next()
            t2, Bt2 = f32r["t2"].next()
            P.op("dve", lambda e, gp=gp, pg=pg, ex=ex, j=j: e.tensor_scalar(out=gp[:], in0=pg[:], scalar1=bg[:, ex, j:j + 1], scalar2=7.0,
                                                                         op0=ALU.add, op1=ALU.min), [Bpg, Bc], [Bgp])
            P.op("act", lambda e, sg=sg, gp=gp: e.activation(out=sg[:], in_=gp[:], func=AF.Sigmoid, scale=1.702), [Bgp], [Bsg])
            P.op("dve", lambda e, up=up, pu=pu, ex=ex, j=j: e.tensor_scalar(out=up[:], in0=pu[:], scalar1=bg[:, ex, 2 + j:3 + j], scalar2=7.0,
                                                                         op0=ALU.add, op1=ALU.min), [Bpu, Bc], [Bup])
            P.op("pool", lambda e, up=up: e.tensor_scalar(out=up[:], in0=up[:], scalar1=-7.0, scalar2=1.0, op0=ALU.max, op1=ALU.add), [Bup], [Bup])
            P.op("pool", lambda e, t1=t1, gp=gp, sg=sg: e.tensor_tensor(out=t1[:], in0=gp[:], in1=sg[:], op=ALU.mult), [Bgp, Bsg], [Bt1])
            P.op("dve", lambda e, t2=t2, up=up, pb=pb: e.tensor_tensor(out=t2[:], in0=up[:], in1=pb[:], op=ALU.mult), [Bup, Bpb], [Bt2])
            P.op("pool", lambda e, t1=t1, t2=t2, ex=ex, j=j: e.tensor_tensor(out=hall[:, ex * 2 + j, :], in0=t1[:], in1=t2[:], op=ALU.mult),
                 [Bt1, Bt2], [Bh[ex]])
    wdr = sb_ring(cx, 2, [128, 2 * NE, 128], BF16, "wd")
    xr = sb_ring(cx, 3, [128, TB], F32, "x")
    rr = sb_ring(cx, 3, [128, TB], F32, "r")
    dor = Ring([cx.buf(f"do{i}") for i in range(3)])
    for m in range(KC):
        wd, Bwd = wdr.next()
        for half in range(2):
            P.dma("pool", wd[:, half * NE:(half + 1) * NE, :],
                  w_down[half * 16:(half + 1) * 16, :, m * 128:(m + 1) * 128].rearrange("e (fc p) j -> p (e fc) j", p=128), [], [Bwd])
        ps, Bp = psg.next()
        for i in range(2 * NE):
            P.mm(ps[:], wd[:, i, :], hall[:, i, :], i == 0, False, [Bwd, Bh[i // 2]], [Bp])
        P.mm(ps[:], bd[:, m * 128:(m + 1) * 128], combTb[:], False, True, [Bw0, BcT], [Bp])
        xt, Bx = xr.next()
        P.dma("sp", xt[:], xT[m, :, ts], [], [Bx])
        P.op("act", lambda e, xt=xt: e.mul(out=xt[:], in_=xt[:], mul=DN_ALPHA), [Bx], [Bx])
        rt, Br = rr.next()
        P.op("dve", lambda e, rt=rt, ps=ps, xt=xt, m=m: e.scalar_tensor_tensor(
            out=rt[:], in0=ps[:], scalar=mod[:, 5, m:m + 1], in1=xt[:], op0=ALU.mult, op1=ALU.add), [Bp, Bx, Bm], [Br])
        P.dma("sp", rT[m, :, ts], rt[:], [Br], [dor.next()])
    cx.finish()


N_MOD = 6
T_ALL = 8192
TLOC = T_ALL // NCORE


def _pk(v):
    return np.ascontiguousarray(np.asarray(v, np.float32).reshape(KC, 128).T)


def _pk6(m):
    return np.ascontiguousarray(np.asarray(m, np.float32).reshape(N_MOD, KC, 128).transpose(2, 0, 1))


def _fm(a, k):
    return np.ascontiguousarray(a.T.reshape(k, 128, -1))


def _dt(nc, name, shape, dt, kind):
    return nc.dram_tensor(name, list(shape), dt, kind=kind).ap()


def _build_adaln(ncols):
    nc = bass.Bass("TRN2", target_bir_lowering=False)
    c_pk = _dt(nc, "c_pk", [128, KC], F32, "ExternalInput")
    w_sh = _dt(nc, "w_sh", [D, ncols], F32, "ExternalInput")
    b_sh = _dt(nc, "b_sh", [1, ncols], F32, "ExternalInput")
    o = _dt(nc, "mod_sh", [1, ncols], F32, "ExternalOutput")
    phase_adaln(nc, c_pk, w_sh, b_sh, o, ncols)
    return nc


def _build_p1():
    nc = bass.Bass("TRN2", target_bir_lowering=False)
    xT = _dt(nc, "xT", [KC, 128, TLOC], F32, "ExternalInput")
    modb = _dt(nc, "modb", [128, N_MOD, KC], F32, "ExternalInput")
    adat = _dt(nc, "adat", [128, N_MOD, KC], F32, "ExternalInput")
    w_g = _dt(nc, "w_g", [D, 8192], F32, "ExternalInput")
    uT_out = _dt(nc, "uT_out", [KC, 128, TLOC], BF16, "ExternalOutput")
    gates = _dt(nc, "gates", [64, 128, TLOC], BF16, "ExternalOutput")
    phase_p1(nc, xT, modb, adat, w_g, uT_out, gates, TLOC, gcol0=0)
    return nc


def _build_k1():
    T = T_ALL
    nc = bass.Bass("TRN2", target_bir_lowering=False)
    UT = _dt(nc, "UT", [KC, 128, T], BF16, "ExternalInput")
    Wc = _dt(nc, "Wc", [D, NWC], F32, "ExternalInput")
    convw = _dt(nc, "convw", [128, 6, 4], F32, "ExternalInput")
    hc = _dt(nc, "hc", [64, 4], F32, "ExternalInput")
    normrep = _dt(nc, "normrep", [64, 8, 256], F32, "ExternalInput")
    cst = _dt(nc, "cst", [128, 5, 128], F32, "ExternalInput")
    biasT = _dt(nc, "biasT", [4, 128, 256], F32, "ExternalInput")
    maskc = _dt(nc, "maskc", [2, 128, 256], F32, "ExternalInput")
    sinkb = _dt(nc, "sinkb", [128, 4], F32, "ExternalInput")
    ident = _dt(nc, "ident", [128, 128], F32, "ExternalInput")
    QKVT = _dt(nc, "QKVT", [6, 128, T], F32, "ExternalOutput")
    QBT = _dt(nc, "QBT", [5, 64, T], F32, "ExternalOutput")
    TMZ = _dt(nc, "TMZ", [T, NTM], F32, "ExternalOutput")
    oA = _dt(nc, "oA", [T, 256], F32, "ExternalOutput")
    oB = _dt(nc, "oB", [T, 256], F32, "ExternalOutput")
    phase_p2a(nc, UT, Wc, QKVT, QBT, TMZ, T)
    phase_p2c(nc, QBT, TMZ, biasT, maskc, sinkb, ident, oB, T)
    phase_p2b(nc, QKVT, TMZ, convw, hc, normrep, cst, oA, T)
    return nc


def _build_k2():
    TL = TLOC
    nc = bass.Bass("TRN2", target_bir_lowering=False)
    I = lambda n, s, d=F32: _dt(nc, n, s, d, "ExternalInput")
    O = lambda n, s, d=F32: _dt(nc, n, s, d, "ExternalOutput")
    oAT = I("oAT", [16, 128, TL]); oBT = I("oBT", [16, 128, TL]); gates = I("gates", [64, 128, TL], BF16)
    wa = I("w_up_a", [2048, D]); wb = I("w_up_b", [2048, D]); wo = I("w_o", [D, D])
    xT = I("xT", [KC, 128, TL]); modb = I("modb", [128, N_MOD, KC]); adat = I("adat", [128, N_MOD, KC])
    l1g = I("ln1g", [128, KC]); l1b = I("ln1b", [128, KC]); l2g = I("ln2g", [128, KC]); l2b = I("ln2b", [128, KC])
    cstk = I("cstk", [128, 5, 128]); eye32 = I("eye32", [32, 32])
    wr = I("w_router", [D, NE]); brep = I("brep", [128, NE]); wgu = I("w_gate_up", [NE, D, 2 * DE]); bgu = I("bgu", [128, NE, 4])
    wd = I("w_down", [NE, DE, D]); bdn = I("b_down", [NE, D])
    r1T = O("r1T", [KC, 128, TL]); x1T = O("x1T", [KC, 128, TL]); u2T = O("u2T", [KC, 128, TL], BF16)
    r2T = O("r2T", [KC, 128, TL]); x2T = O("x2T", [KC, 128, TL]); u3T = O("u3T", [KC, 128, TL], BF16)
    for t0 in range(0, TL, 512):
        phase_p3(nc, oAT, oBT, gates, wa, wb, wo, xT, modb, adat, r1T, t0, 512)
    phase_ln(nc, r1T, l1g, l1b, modb, adat, 3, x1T, u2T, cstk, TL)
    for t0 in range(0, TL, 256):
        phase_moe(nc, u2T, x1T, wr, brep, wgu, bgu, wd, bdn, modb, adat, cstk, eye32, r2T, t0, 256)
    phase_ln(nc, r2T, l2g, l2b, modb, adat, 0, x2T, u3T, cstk, TL)
    return nc


def _band_tables(rel_bias):
    import math
    i = np.arange(128)[:, None]
    j = np.arange(256)[None, :]
    d = i + 128 - j
    dc = np.clip(d, 0, 127)
    df = np.maximum(dc, 1).astype(np.float32)
    large = 16 + (np.log(df / 16) / math.log(128 / 16) * 16).astype(np.int32)
    large = np.minimum(large, 31)
    bucket = np.where(dc < 16, dc, large)
    bias_all = np.asarray(rel_bias, np.float32)[bucket]
    valid = (d >= 0) & (d < 128)
    m0 = np.where(valid, 0.0, -30000.0).astype(np.float32)
    m1 = m0.copy()
    m1[:, :128] = -30000.0
    return bias_all, np.stack([m0, m1])


def kernel(x, c, w_ada, b_ada, ada_table, rel_bias, w_in, conv_w, a_log, dt_bias, norm_a, sinks,
           w_up_a, w_up_b, w_o, ln1_g, ln1_b, w_router, b_router, w_gate_up, b_gate_up, w_down,
           b_down, ln2_g, ln2_b):
    f32 = np.float32
    cores = list(range(NCORE))
    x = np.asarray(x, f32)
    ncols = N_MOD * D // NCORE
    c_pk = _pk(np.asarray(c, f32)[0])
    nc0 = _build_adaln(ncols)
    in0 = [{"c_pk": c_pk,
            "w_sh": np.ascontiguousarray(w_ada[:, r * ncols:(r + 1) * ncols]),
            "b_sh": np.ascontiguousarray(np.asarray(b_ada, f32)[None, r * ncols:(r + 1) * ncols])} for r in cores]
    r0 = run_bass_kernel_spmd(nc0, in0, core_ids=cores)
    mod_base = np.concatenate([r0.results[r]["mod_sh"][0] for r in cores]).reshape(N_MOD, D)
    modb = _pk6(mod_base)
    bias_all, maskc = _band_tables(rel_bias)
    cst = k1_consts()
    ident = np.eye(128, dtype=f32)
    eye32 = np.eye(32, dtype=f32)
    nc1, nck1, nck2 = _build_p1(), _build_k1(), _build_k2()
    xT = [np.ascontiguousarray(x[0, r * TLOC:(r + 1) * TLOC, :].T.reshape(KC, 128, TLOC)) for r in cores]
    for l in range(4):
        adat = _pk6(ada_table[l])
        Wl = np.asarray(w_in[l], f32)
        w_g = np.ascontiguousarray(Wl[:, OFF_GA:OFF_GA + 8192])
        r1 = run_bass_kernel_spmd(nc1, [{"xT": xT[r], "modb": modb, "adat": adat, "w_g": w_g} for r in cores], core_ids=cores)
        UT = np.ascontiguousarray(np.concatenate([r1.results[r]["uT_out"] for r in cores], axis=2))
        gates = [r1.results[r]["gates"] for r in cores]
        del w_g
        cw = np.asarray(conv_w[l], f32)
        ins = []
        for hg in cores:
            cols = []
            for base in (OFF_QA, OFF_KA, OFF_VA):
                for h in range(2):
                    cols.append(np.arange(base + (2 * hg + h) * 128, base + (2 * hg + h + 1) * 128))
            cols.append(np.arange(OFF_QB + hg * 256, OFF_QB + (hg + 1) * 256))
            cols.append(np.arange(OFF_KB + hg * 64, OFF_KB + (hg + 1) * 64))
            cols.append(np.arange(OFF_ZA + hg * 256, OFF_ZA + (hg + 1) * 256))
            cols.append(np.arange(OFF_VB + hg * 64, OFF_VB + (hg + 1) * 64))
            cols.append(np.arange(OFF_BA + 2 * hg, OFF_BA + 2 * hg + 2))
            cols.append(np.arange(OFF_AA + 2 * hg, OFF_AA + 2 * hg + 2))
            cols = np.concatenate(cols)
            Wc = np.ascontiguousarray(Wl[:, cols])
            chb = [kind * 2048 + (2 * hg + h) * 128 for kind in range(3) for h in range(2)]
            convw = np.ascontiguousarray(np.stack([cw[:, b0:b0 + 128] for b0 in chb], 0).transpose(2, 0, 1))
            hcv = np.concatenate([np.asarray(a_log[l], f32)[2 * hg:2 * hg + 2], np.asarray(dt_bias[l], f32)[2 * hg:2 * hg + 2]])
            heads = [4 * hg + k for k in range(4)]
            ins.append({"UT": UT, "Wc": Wc, "convw": convw,
                        "hc": np.ascontiguousarray(np.broadcast_to(hcv[None], (64, 4))),
                        "normrep": np.ascontiguousarray(np.broadcast_to(np.tile(np.asarray(norm_a[l], f32), 2)[None, None], (64, 8, 256))),
                        "cst": cst, "biasT": np.ascontiguousarray(bias_all[:, :, heads].transpose(2, 0, 1)), "maskc": maskc,
                        "sinkb": np.ascontiguousarray(np.broadcast_to(np.asarray(sinks[l], f32)[heads][None], (128, 4))),
                        "ident": ident})
        rk1 = run_bass_kernel_spmd(nck1, ins, core_ids=cores)
        oA = np.concatenate([rk1.results[r]["oA"] for r in cores], axis=1)
        oB = np.concatenate([rk1.results[r]["oB"] for r in cores], axis=1)
        del ins, rk1, UT
        shared = {"w_up_a": np.asarray(w_up_a[l], f32), "w_up_b": np.asarray(w_up_b[l], f32), "w_o": np.asarray(w_o[l], f32),
                  "modb": modb, "adat": adat, "ln1g": _pk(ln1_g[l]), "ln1b": _pk(ln1_b[l]), "ln2g": _pk(ln2_g[l]), "ln2b": _pk(ln2_b[l]),
                  "cstk": cst, "eye32": eye32, "w_router": np.asarray(w_router[l], f32),
                  "brep": np.ascontiguousarray(np.broadcast_to(np.asarray(b_router[l], f32)[None], (128, NE))),
                  "w_gate_up": np.asarray(w_gate_up[l], f32),
                  "bgu": np.ascontiguousarray(np.asarray(b_gate_up[l], f32).reshape(NE, 4, 128).transpose(2, 0, 1)),
                  "w_down": np.asarray(w_down[l], f32), "b_down": np.asarray(b_down[l], f32)}
        ins = []
        for r in cores:
            sl = slice(r * TLOC, (r + 1) * TLOC)
            d_ = dict(shared)
            d_.update({"oAT": _fm(oA[sl], 16), "oBT": _fm(oB[sl], 16), "gates": gates[r], "xT": xT[r]})
            ins.append(d_)
        rk2 = run_bass_kernel_spmd(nck2, ins, core_ids=cores)
        xT = [np.ascontiguousarray(rk2.results[r]["x2T"]) for r in cores]
        del ins, rk2, oA, oB, gates
    out = np.concatenate([xT[r].reshape(D, TLOC).T for r in cores], axis=0)
    return np.ascontiguousarray(out[None]).astype(np.float32)
```

```python
import numpy as np
from contextlib import ExitStack
from concourse.bass_utils import run_bass_kernel_spmd
import concourse.bass as bass
import concourse.mybir as mybir

F32 = mybir.dt.float32
BF16 = mybir.dt.bfloat16
AF = mybir.ActivationFunctionType
ALU = mybir.AluOpType
AX = mybir.AxisListType

STRICT_SAME_ENGINE = True


class Buf:
    __slots__ = ("name", "last_w", "readers", "sem")

    def __init__(self, name):
        self.name = name
        self.last_w = None
        self.readers = []
        self.sem = None


class Prog:
    ENGS = ("pe", "act", "dve", "pool", "sp")

    def __init__(self, nc):
        self.nc = nc
        self.ins = []
        self.dma_sem_count = {}
        self.sems = []
        self._sem_ctx = []

    _uid = [0]

    def new_sem(self, name):
        Prog._uid[0] += 1
        s = self.nc.alloc_semaphore(name=f"{name}_{Prog._uid[0]}")
        self.sems.append(s)
        return s

    def release(self):
        nc = self.nc
        nc.all_engine_barrier()
        nc.clear_and_free_semaphores(self.sems)
        nc.all_engine_barrier()
        self.sems = []

    def buf(self, name):
        return Buf(name)

    def op(self, eng, fn, reads=(), writes=(), dma=False):
        i = len(self.ins)
        deps = set()
        for b in reads:
            if b.last_w is not None:
                deps.add(b.last_w)
        for b in writes:
            if b.last_w is not None:
                deps.add(b.last_w)
            deps.update(b.readers)
        tok = None
        if dma:
            b0 = writes[0]
            if b0.sem is None:
                b0.sem = self.new_sem("d_" + b0.name)
                self.dma_sem_count[b0.sem] = 0
            self.dma_sem_count[b0.sem] += 16
            tok = (b0.sem, self.dma_sem_count[b0.sem])
        self.ins.append([eng, fn, deps, dma, tok, False])
        for b in reads:
            b.readers.append(i)
        for b in writes:
            b.last_w = i
            b.readers = []
        return i

    def dma(self, q, out, in_, reads, writes, **kw):
        return self.op(q, lambda e: e.dma_start(out=out, in_=in_, **kw), reads, writes, dma=True)

    def mm(self, out, lhsT, rhs, start, stop, reads, writes, **kw):
        return self.op("pe", lambda e: e.matmul(out, lhsT, rhs, start=start, stop=stop, **kw), reads, writes)

    def emit(self):
        nc = self.nc
        ins = self.ins
        for i, (eng, fn, deps, dma, tok, sig) in enumerate(ins):
            for d in deps:
                p = ins[d]
                if p[3]:
                    continue
                if p[0] != eng or (STRICT_SAME_ENGINE and eng != "pe"):
                    p[5] = True
        esem = {e: self.new_sem("e_" + e) for e in self.ENGS}
        cnt = {e: 0 for e in self.ENGS}
        for it in ins:
            if not it[3] and it[5]:
                cnt[it[0]] += 1
                it[4] = (esem[it[0]], cnt[it[0]])
        per = {e: [] for e in self.ENGS}
        for i, it in enumerate(ins):
            per[it[0]].append(i)

        def run(e, lst):
            waited = {}
            for i in lst:
                eng, fn, deps, dma, tok, sig = ins[i]
                need = {}
                for d in deps:
                    t = ins[d][4]
                    if t is None:
                        continue
                    s, v = t
                    if waited.get(s, 0) >= v:
                        continue
                    if need.get(s, 0) < v:
                        need[s] = v
                for s, v in need.items():
                    e.wait_ge(s, v)
                    waited[s] = v
                h = fn(e)
                if dma:
                    h.then_inc(tok[0], 16)
                elif sig:
                    h.then_inc(tok[0], 1)

        with nc.Block() as block:
            @block.tensor
            def _(e):
                run(e, per["pe"])

            @block.scalar
            def _(e):
                run(e, per["act"])

            @block.vector
            def _(e):
                run(e, per["dve"])

            @block.gpsimd
            def _(e):
                run(e, per["pool"])

            @block.sync
            def _(e):
                run(e, per["sp"])
                for s, v in self.dma_sem_count.items():
                    e.wait_ge(s, v)
                for en in self.ENGS:
                    if en != "sp" and cnt[en] > 0:
                        e.wait_ge(esem[en], cnt[en])


D = 4096
KC = 32
NCORE = 8
HA, DKA, DVA = 16, 128, 128
HB, HBKV, DHB = 32, 8, 64
WA = HA * DKA
C_IN = 19488
OFF_QA, OFF_KA, OFF_VA, OFF_ZA = 0, 2048, 4096, 6144
OFF_BA, OFF_AA = 8192, 8208
OFF_QB, OFF_KB, OFF_VB = 8224, 10272, 10784
OFF_GA, OFF_GB = 11296, 15392
NE, DE = 32, 256
DN_ALPHA = 8.0 ** 0.25
LN_EPS = 1e-5
NORM_EPS = 1e-6


class Ctx:
    def __init__(self, nc):
        self.nc = nc
        self.P = Prog(nc)
        self.es = ExitStack()
        self.n = 0

    _gn = [0]

    def sb(self, shape, dt, name=None):
        Ctx._gn[0] += 1
        t = self.es.enter_context(self.nc.sbuf_tensor(f"{name or 's'}_{Ctx._gn[0]}", list(shape), dt))
        return t

    def ps(self, shape, dt=F32, name=None):
        Ctx._gn[0] += 1
        return self.es.enter_context(self.nc.psum_tensor(f"{name or 'p'}_{Ctx._gn[0]}", list(shape), dt))

    def buf(self, name):
        return self.P.buf(name)

    def finish(self):
        self.P.emit()
        self.es.close()
        self.P.release()


class Ring:
    def __init__(self, items):
        self.items = items
        self.i = 0

    def next(self):
        it = self.items[self.i % len(self.items)]
        self.i += 1
        return it


def sb_ring(cx, n, shape, dt, name):
    return Ring([(cx.sb(shape, dt, name), cx.buf(f"{name}{i}")) for i in range(n)])


def ps_ring(cx, n, shape, name, dt=F32):
    return Ring([(cx.ps(shape, dt, name), cx.buf(f"{name}{i}")) for i in range(n)])


def phase_adaln(nc, c_pk, w_sh, b_sh, out, ncols):
    cx = Ctx(nc)
    P = cx.P
    ct = cx.sb([128, KC], F32, "c")
    cs = cx.sb([128, KC], F32, "cs")
    bt = cx.sb([1, ncols], F32, "b")
    ot = cx.sb([1, ncols], F32, "o")
    Bc, Bcs, Bb, Bo, Bout = [cx.buf(n) for n in ("c", "cs", "b", "o", "out")]
    P.dma("sp", ct[:], c_pk, [], [Bc])
    P.dma("sp", bt[:], b_sh, [], [Bb])
    P.op("act", lambda e: e.activation(out=cs[:], in_=ct[:], func=AF.Silu), [Bc], [Bcs])
    nb = ncols // 512
    pss = [(cx.ps([1, 512], F32, "acc"), cx.buf(f"acc{i}")) for i in range(nb)]
    wr = sb_ring(cx, 3, [128, ncols], F32, "w")
    for kc in range(KC):
        wt, Bw = wr.next()
        P.dma("sp", wt[:], w_sh[kc * 128:(kc + 1) * 128, :], [], [Bw])
        for j in range(nb):
            ps, Bp = pss[j]
            P.mm(ps[:], cs[:, kc:kc + 1], wt[:, j * 512:(j + 1) * 512], kc == 0, kc == KC - 1, [Bcs, Bw], [Bp])
    for j in range(nb):
        ps, Bp = pss[j]
        P.op("dve", lambda e, ps=ps, j=j: e.tensor_tensor(out=ot[:, j * 512:(j + 1) * 512], in0=ps[:],
                                                         in1=bt[:, j * 512:(j + 1) * 512], op=ALU.add),
             [Bp, Bb], [Bo])
    P.dma("sp", out, ot[:], [Bo], [Bout])
    cx.finish()


def stream_linear(cx, W, col0, n_m, n_kc, act, TL, wring, psring, epilogue, row0=0, CW=256):
    P = cx.P
    ntb = TL // 512
    mpw = CW // 128
    for wt_i in range(n_m // mpw):
        wt, Bw = wring.next()
        c0 = col0 + wt_i * CW
        src = W[row0:row0 + n_kc * 128, c0:c0 + CW].rearrange("(kc p) j -> p kc j", p=128)
        P.dma("pool", wt[:, 0:n_kc, 0:CW], src, [], [Bw])
        for mi in range(mpw):
            m = wt_i * mpw + mi
            ps, Bp = psring.next()
            for tb in range(ntb):
                for kc in range(n_kc):
                    a_ap, a_bufs = act(kc, tb * 512, (tb + 1) * 512)
                    P.mm(ps[:, tb * 512:(tb + 1) * 512], wt[:, kc, mi * 128:(mi + 1) * 128],
                         a_ap, kc == 0, kc == n_kc - 1, [Bw] + a_bufs, [Bp])
            epilogue(m, ps, Bp)


def load_mod(cx, modb, adat, idx_list):
    P = cx.P
    a = cx.sb([128, 6, KC], F32, "moda")
    b = cx.sb([128, 6, KC], F32, "modb")
    m = cx.sb([128, 6, KC], F32, "mod")
    Ba, Bb, Bm = cx.buf("moda"), cx.buf("modb"), cx.buf("mod")
    P.dma("sp", a[:], modb, [], [Ba])
    P.dma("sp", b[:], adat, [], [Bb])
    P.op("dve", lambda e: e.tensor_tensor(out=m[:], in0=a[:], in1=b[:], op=ALU.add), [Ba, Bb], [Bm])
    for i in idx_list:
        P.op("dve", lambda e, i=i: e.tensor_scalar_add(out=m[:, i, :], in0=m[:, i, :], scalar1=1.0), [Bm], [Bm])
    return m, Bm


def phase_p1(nc, xT, modb, adat, w_in, uT_out, gates_out, TL, gcol0=OFF_GA):
    cx = Ctx(nc)
    P = cx.P
    mod, Bm = load_mod(cx, modb, adat, [1])
    uT = cx.sb([128, KC, TL], BF16, "uT")
    BuT = [cx.buf(f"uT{k}") for k in range(KC)]
    Buo = cx.buf("uTout")
    xr = sb_ring(cx, 3, [128, TL], F32, "x")
    for kc in range(KC):
        xt, Bx = xr.next()
        P.dma("sp", xt[:], xT[kc], [], [Bx])
        eng = "dve" if kc % 2 == 0 else "act"
        if eng == "dve":
            P.op("dve", lambda e, xt=xt, kc=kc: e.tensor_scalar(
                out=uT[:, kc, :], in0=xt[:], scalar1=mod[:, 1, kc:kc + 1], scalar2=mod[:, 0, kc:kc + 1],
                op0=ALU.mult, op1=ALU.add), [Bx, Bm], [BuT[kc]])
        else:
            P.op("act", lambda e, xt=xt, kc=kc: e.activation(
                out=uT[:, kc, :], in_=xt[:], func=AF.Identity, bias=mod[:, 0, kc:kc + 1],
                scale=mod[:, 1, kc:kc + 1]), [Bx, Bm], [BuT[kc]])
    P.dma("sp", uT_out.rearrange("kc p t -> p kc t"), uT[:], BuT, [Buo])
    wring = sb_ring(cx, 3, [128, KC, 256], BF16, "w")
    psring = ps_ring(cx, 4, [128, TL], "ps")
    gr = sb_ring(cx, 3, [128, TL], BF16, "g")
    gor = Ring([cx.buf(f"go{i}") for i in range(4)])

    def epi(m, ps, Bp):
        gt, Bg = gr.next()
        P.op("act", lambda e: e.activation(out=gt[:], in_=ps[:], func=AF.Sigmoid), [Bp], [Bg])
        P.dma("sp", gates_out[m], gt[:], [Bg], [gor.next()])

    stream_linear(cx, w_in, gcol0, 64, KC, lambda kc, a, b: (uT[:, kc, a:b], [BuT[kc]]), TL, wring, psring, epi)
    cx.finish()


NFM = 1088
NTM = 324
NWC = NFM + NTM


def phase_p2a(nc, UT_all, Wc, QKVT, QBT, TMZ, T):
    cx = Ctx(nc)
    P = cx.P
    wfm = cx.sb([128, KC, NFM], BF16, "wfm")
    wtm = cx.sb([128, KC, NTM], BF16, "wtm")
    Bw = cx.buf("w")

    def wsrc(c0, n):
        return Wc[:, c0:c0 + n].rearrange("(kc p) j -> p kc j", p=128)
    for c0 in range(0, NFM, 128):
        n = min(128, NFM - c0)
        P.dma("pool", wfm[:, :, c0:c0 + n], wsrc(c0, n), [], [Bw])
    for c0 in range(0, NTM, 128):
        n = min(128, NTM - c0)
        P.dma("pool", wtm[:, :, c0:c0 + n], wsrc(NFM + c0, n), [], [Bw])
    ur = sb_ring(cx, 2, [128, KC, 512], BF16, "ut")
    psr = ps_ring(cx, 3, [128, 512], "psf")
    pst = ps_ring(cx, 2, [128, NTM], "pst")
    fo = sb_ring(cx, 3, [128, 512], F32, "fo")
    to = sb_ring(cx, 3, [128, NTM], F32, "to")
    dor = Ring([cx.buf(f"do{i}") for i in range(4)])
    groups = [(g * 128, 128) for g in range(6)] + [(768 + 64 * g, 64) for g in range(5)]
    for tb in range(T // 512):
        ut, But = ur.next()
        P.dma("sp", ut[:], UT_all[:, :, tb * 512:(tb + 1) * 512].rearrange("kc p t -> p kc t"), [], [But])
        for g, (c0, m) in enumerate(groups):
            ps, Bp = psr.next()
            for kc in range(KC):
                P.mm(ps[0:m, :], wfm[:, kc, c0:c0 + m], ut[:, kc, :], kc == 0, kc == KC - 1, [Bw, But], [Bp])
            ft, Bf = fo.next()
            if g % 2 == 0:
                P.op("act", lambda e, ft=ft, ps=ps, m=m: e.copy(out=ft[0:m, :], in_=ps[0:m, :]), [Bp], [Bf])
            else:
                P.op("act", lambda e, ft=ft, ps=ps, m=m: e.copy(out=ft[0:m, :], in_=ps[0:m, :]), [Bp], [Bf])
            dst = QKVT[g, :, tb * 512:(tb + 1) * 512] if g < 6 else QBT[g - 6, :, tb * 512:(tb + 1) * 512]
            P.dma("sp", dst, ft[0:m, :], [Bf], [dor.next()])
        for tt in range(4):
            ps, Bp = pst.next()
            for kc in range(KC):
                P.mm(ps[:], ut[:, kc, tt * 128:(tt + 1) * 128], wtm[:, kc, :], kc == 0, kc == KC - 1, [But, Bw], [Bp])
            tt_, Bt = to.next()
            P.op("act", lambda e, tt_=tt_, ps=ps: e.copy(out=tt_[:], in_=ps[:]), [Bp], [Bt])
            j = tb * 4 + tt
            P.dma("sp", TMZ[j * 128:(j + 1) * 128, :], tt_[:], [Bt], [dor.next()])
    cx.finish()


def phase_p2c(nc, QBT, TMZ, biasT, maskc, sinkb, ident, oB, T):
    cx = Ctx(nc)
    P = cx.P
    NT = T // 128
    qk = cx.sb([64, 5, T], F32, "qk")
    V = cx.sb([128, NT + 1, 64], F32, "V")
    bm = cx.sb([128, 4, 256], F32, "bm")
    bm0 = cx.sb([128, 4, 256], F32, "bm0")
    mk = cx.sb([128, 2, 256], F32, "mk")
    sk = cx.sb([128, 4], F32, "sk")
    idt = cx.sb([128, 128], F32, "id")
    Bq, Bv, Bc, Bbm, Bbm0 = [cx.buf(n) for n in "q v c bm bm0".split()]
    for g in range(5):
        P.dma("sp", qk[:, g, :], QBT[g], [], [Bq])
    P.op("pool", lambda e: e.memset(V[:, 0, :], 0.0), [], [Bv])
    P.dma("sp", V[:, 1:NT + 1, :], TMZ[:, 256:320].rearrange("(j p) c -> p j c", p=128), [], [Bv])
    P.dma("sp", bm[:], biasT.rearrange("h p k -> p h k"), [], [Bc])
    P.dma("sp", mk[:], maskc.rearrange("h p k -> p h k"), [], [Bc])
    P.dma("sp", sk[:], sinkb, [], [Bc])
    P.dma("sp", idt[:], ident, [], [Bc])
    for j in range(4):
        P.op("dve", lambda e, j=j: e.tensor_tensor(out=bm0[:, j, :], in0=bm[:, j, :], in1=mk[:, 1, :], op=ALU.add),
             [Bc], [Bbm0])
    bmm = cx.sb([128, 4, 256], F32, "bmm")
    for j in range(4):
        P.op("dve", lambda e, j=j: e.tensor_tensor(out=bmm[:, j, :], in0=bm[:, j, :], in1=mk[:, 0, :], op=ALU.add),
             [Bc], [Bbm])
    pss = ps_ring(cx, 2, [128, 256], "pss")
    pst = ps_ring(cx, 2, [128, 256], "pst")
    pso = ps_ring(cx, 2, [128, 64], "pso")
    sr = sb_ring(cx, 3, [128, 256], F32, "s")
    pr = sb_ring(cx, 3, [128, 256], F32, "p")
    ptr = sb_ring(cx, 3, [128, 256], F32, "pt")
    st = sb_ring(cx, 4, [128, 8], F32, "st")
    osb = sb_ring(cx, 2, [128, 256], F32, "osb")
    dor = Ring([cx.buf(f"do{i}") for i in range(2)])
    for n in range(NT):
        ot, Bo = osb.next()
        for j in range(4):
            ps, Bp = pss.next()
            if n == 0:
                P.mm(ps[:, 128:256], qk[:, j, 0:128], qk[:, 4, 0:128], True, True, [Bq], [Bp])
                P.mm(ps[:, 0:128], qk[:, j, 0:128], qk[:, 4, 0:128], True, True, [Bq], [Bp])
            else:
                P.mm(ps[:], qk[:, j, n * 128:(n + 1) * 128], qk[:, 4, (n - 1) * 128:(n + 1) * 128], True, True, [Bq], [Bp])
            s, Bs = sr.next()
            bsrc = bm0 if n == 0 else bmm
            P.op("dve", lambda e, s=s, ps=ps, bsrc=bsrc, j=j: e.scalar_tensor_tensor(
                out=s[:], in0=ps[:], scalar=0.125, in1=bsrc[:, j, :], op0=ALU.mult, op1=ALU.add),
                [Bp, Bbm, Bbm0], [Bs])
            sv, Bsv = st.next()
            P.op("dve", lambda e, s=s, sv=sv: e.reduce_max(out=sv[:, 0:1], in_=s[:], axis=AX.X), [Bs], [Bsv])
            P.op("dve", lambda e, sv=sv, j=j: e.tensor_scalar(out=sv[:, 1:2], in0=sv[:, 0:1], scalar1=sk[:, j:j + 1],
                                                           scalar2=-1.0, op0=ALU.max, op1=ALU.mult), [Bsv, Bc], [Bsv])
            p, Bpp = pr.next()
            P.op("act", lambda e, p=p, s=s, sv=sv: e.activation(out=p[:], in_=s[:], func=AF.Exp, bias=sv[:, 1:2],
                                                              scale=1.0), [Bs, Bsv], [Bpp])
            P.op("dve", lambda e, p=p, sv=sv: e.reduce_sum(out=sv[:, 2:3], in_=p[:], axis=AX.X), [Bpp, Bsv], [Bsv])
            P.op("act", lambda e, sv=sv, j=j: e.activation(out=sv[:, 3:4], in_=sk[:, j:j + 1], func=AF.Exp,
                                                         bias=sv[:, 1:2], scale=1.0), [Bsv, Bc], [Bsv])
            P.op("dve", lambda e, sv=sv: e.tensor_tensor(out=sv[:, 4:5], in0=sv[:, 2:3], in1=sv[:, 3:4], op=ALU.add),
                 [Bsv], [Bsv])
            P.op("dve", lambda e, sv=sv: e.reciprocal(out=sv[:, 5:6], in_=sv[:, 4:5]), [Bsv], [Bsv])
            pt_ps, Bptp = pst.next()
            for hf in range(2):
                P.mm(pt_ps[:, hf * 128:(hf + 1) * 128], p[:, hf * 128:(hf + 1) * 128], idt[:], True, True, [Bpp, Bc], [Bptp])
            pt, Bpt = ptr.next()
            P.op("act", lambda e, pt=pt, pt_ps=pt_ps: e.copy(out=pt[:], in_=pt_ps[:]), [Bptp], [Bpt])
            po, Bpo = pso.next()
            for hf in range(2):
                P.mm(po[:], pt[:, hf * 128:(hf + 1) * 128], V[:, n + hf, :], hf == 0, hf == 1, [Bpt, Bv], [Bpo])
            P.op("dve", lambda e, ot=ot, po=po, sv=sv, j=j: e.tensor_scalar(
                out=ot[:, j * 64:(j + 1) * 64], in0=po[:], scalar1=sv[:, 5:6], scalar2=None, op0=ALU.mult),
                [Bpo, Bsv], [Bo])
        P.dma("sp", oB[n * 128:(n + 1) * 128, :], ot[:], [Bo], [dor.next()])
    cx.finish()


def phase_p2b(nc, QKVT, TMZ, convw, hc, normrep, cst, oA, T):
    STAGE = 9
    cx = Ctx(nc)
    P = cx.P
    NCH = T // 64
    NB = T // 512
    Bc = cx.buf("c")
    cw = cx.sb([128, 6, 4], F32, "cw")
    hcs = cx.sb([64, 4], F32, "hc")
    nrep = cx.sb([64, 8, 256], F32, "nrep")
    cs_ = cx.sb([128, 5, 128], F32, "cst")
    P.dma("sp", cw[:], convw, [], [Bc])
    P.dma("sp", hcs[:], hc, [], [Bc])
    P.dma("sp", nrep[:], normrep, [], [Bc])
    P.dma("sp", cs_[:], cst, [], [Bc])
    ident = cs_[:, 0, :]
    ones = cs_[:, 1, :]
    tri2 = cs_[0:64, 2, :]
    mask2 = cs_[0:64, 3, :]
    strict = cs_[0:64, 4, 0:64]
    id64 = cs_[0:64, 0, 0:64]
    ab = cx.sb([64, NCH, 4], F32, "ab")
    Bab = cx.buf("ab")
    for c0 in range(0, NCH, 16):
        c1 = min(NCH, c0 + 16)
        P.dma("sp", ab[:, c0:c1, :], TMZ[c0 * 64:c1 * 64, 320:324].rearrange("(n p) c -> p n c", p=64), [], [Bab])
    beta = cx.sb([64, NCH, 2], F32, "beta")
    gg = cx.sb([64, NCH, 2], F32, "gg")
    x1 = cx.sb([64, NCH, 2], F32, "x1")
    negA = cx.sb([64, 2], F32, "negA")
    Gc = cx.sb([64, NCH, 2], F32, "Gc")
    expG = cx.sb([64, NCH, 2], F32, "expG")
    edec = cx.sb([64, NCH, 2], F32, "edec")
    bexp = cx.sb([64, NCH, 2], F32, "bexp")
    eGl = cx.sb([128, NCH, 2], F32, "eGl")
    Bpre = cx.buf("pre")
    P.op("act", lambda e: e.activation(out=beta[:], in_=ab[:, :, 0:2], func=AF.Sigmoid), [Bab], [Bpre])
    P.op("act", lambda e: e.activation(out=negA[:], in_=hcs[:, 0:2], func=AF.Exp), [Bc], [Bpre])
    P.op("dve", lambda e: e.tensor_scalar(out=negA[:], in0=negA[:], scalar1=-1.0, scalar2=None, op0=ALU.mult), [Bpre], [Bpre])
    for h in range(2):
        P.op("dve", lambda e, h=h: e.tensor_scalar(out=x1[:, :, h], in0=ab[:, :, 2 + h], scalar1=hcs[:, 2 + h:3 + h],
                                                 scalar2=None, op0=ALU.add), [Bab, Bc], [Bpre])
    P.op("act", lambda e: e.activation(out=x1[:], in_=x1[:], func=AF.Exp), [Bpre], [Bpre])
    P.op("dve", lambda e: e.tensor_scalar(out=x1[:], in0=x1[:], scalar1=1.0, scalar2=None, op0=ALU.add), [Bpre], [Bpre])
    P.op("act", lambda e: e.activation(out=x1[:], in_=x1[:], func=AF.Ln), [Bpre], [Bpre])
    for h in range(2):
        P.op("dve", lambda e, h=h: e.tensor_scalar(out=gg[:, :, h], in0=x1[:, :, h], scalar1=negA[:, h:h + 1],
                                                 scalar2=None, op0=ALU.mult), [Bpre], [Bpre])
    banks = [(cx.ps([128, 512], F32, f"bk{i}")) for i in range(6)]
    Bbank = [cx.buf(f"bank{i}") for i in range(6)]
    Bb0, Bb1 = Bbank[0], Bbank[1]
    ggf = gg[:].rearrange("p n h -> p (n h)")
    P.mm(banks[0][0:64, 0:NCH * 2], tri2[:, 0:64], ggf, True, True, [Bc, Bpre], [Bb0])
    P.mm(banks[1][:, 0:NCH * 2], ones[0:64, :], ggf, True, True, [Bc, Bpre], [Bb1])
    Gcf = Gc[:].rearrange("p n h -> p (n h)")
    P.op("act", lambda e: e.copy(out=Gcf, in_=banks[0][0:64, 0:NCH * 2]), [Bb0], [Bpre])
    P.op("act", lambda e: e.activation(out=expG[:].rearrange("p n h -> p (n h)"), in_=Gcf, func=AF.Exp), [Bpre], [Bpre])
    P.op("dve", lambda e: e.tensor_tensor(out=edec[:].rearrange("p n h -> p (n h)"), in0=banks[1][0:64, 0:NCH * 2], in1=Gcf,
                                        op=ALU.subtract), [Bb1, Bpre], [Bpre])
    P.op("act", lambda e: e.activation(out=edec[:], in_=edec[:], func=AF.Exp), [Bpre], [Bpre])
    P.op("act", lambda e: e.activation(out=eGl[:].rearrange("p n h -> p (n h)"), in_=banks[1][:, 0:NCH * 2], func=AF.Exp),
         [Bb1], [Bpre])
    P.op("dve", lambda e: e.tensor_tensor(out=bexp[:], in0=beta[:], in1=expG[:], op=ALU.mult), [Bpre], [Bpre])
    S = [[cx.sb([128, 128], F32, f"S{h}{i}") for i in range(2)] for h in range(2)]
    BS = [[cx.buf(f"S{h}{i}") for i in range(2)] for h in range(2)]
    for h in range(2):
        P.op("pool", lambda e, h=h: e.memset(S[h][0][:], 0.0), [], [BS[h][0]])
    scur = [0, 0]
    rawr = sb_ring(cx, 2, [128, 6, 515], F32, "raw")
    accr = sb_ring(cx, 2, [128, 512], F32, "acc")
    csr = sb_ring(cx, 2, [128, 6, 512], F32, "cs")
    sqr = sb_ring(cx, 2, [128, 512], F32, "sq")
    rnr = sb_ring(cx, 2, [128, 512], F32, "rn")
    qknr = sb_ring(cx, 2, [128, 4, 512], F32, "qkn")
    ztr = sb_ring(cx, 2, [64, 8, 256], F32, "zt")
    obr = sb_ring(cx, 2, [64, 16, 128], F32, "ob")
    osq = cx.sb([64, 16, 128], F32, "osq")
    Bosq = cx.buf("osq")
    nst = sb_ring(cx, 2, [64, 32], F32, "nst")
    ogr = sb_ring(cx, 2, [64, 8, 256], F32, "og")
    dor = Ring([cx.buf(f"do{i}") for i in range(2)])

    def psr(bank, col0, w, rows):
        return Ring([(banks[bank][0:rows, col0:col0 + w], Bbank[bank])])
    r_tk = psr(1, 0, 256, 64)
    r_gr = psr(2, 0, 128, 64)
    r_kq = psr(2, 256, 128, 64)
    r_tt = psr(3, 0, 128, 64)
    r_sq = psr(4, 0, 128, 64)
    r_pq = psr(5, 0, 128, 64)
    r_wt = psr(3, 128, 64, 128)
    r_u = psr(3, 256, 128, 64)
    r_ws = psr(4, 256, 256, 64)
    r_iv = psr(5, 256, 128, 64)
    r_kv = psr(1, 256, 128, 128)
    Bssq = Bbank[0]

    def sring(n, shape, name):
        return sb_ring(cx, n, shape, F32, name)
    s_kbg, s_kdec, s_vb = sring(3, [128, 128], "kbg"), sring(3, [128, 128], "kdec"), sring(3, [64, 128], "vb")
    s_gbt, s_eD, s_Dm, s_X = sring(2, [64, 64], "gbt"), sring(2, [64, 128], "eD"), sring(2, [64, 128], "Dm"), sring(2, [64, 128], "X")
    s_UL = sring(4, [64, 128], "UL")
    s_PQ = sring(4, [128, 128], "PQ")
    s_iT = sring(3, [64, 64], "iT")
    s_wT, s_u = sring(3, [128, 64], "wT"), sring(3, [64, 128], "u")
    s_vn, s_oq = sring(3, [128, 128], "vn"), sring(3, [64, 128], "oq")
    for rg in (s_kbg, s_kdec, s_PQ, s_vn):
        for (tl, Bt_) in rg.items:
            P.op("pool", lambda e, tl=tl: e.memset(tl[64:128, :], 0.0), [], [Bt_])
    I2 = cs_[0:64, 4, :]
    i2t = cx.sb([64, 128], F32, "I2")
    BI2 = cx.buf("I2")
    P.op("pool", lambda e: e.tensor_copy(out=i2t[:, 0:64], in_=id64), [Bc], [BI2])
    P.op("pool", lambda e: e.tensor_copy(out=i2t[:, 64:128], in_=id64), [Bc], [BI2])

    for b in range(NB if STAGE >= 2 else 0):
        raw, Braw = rawr.next()
        if b == 0:
            P.op("pool", lambda e, raw=raw: e.memset(raw[:, :, 0:3], 0.0), [], [Braw])
            P.dma("sp", raw[:, :, 3:515], QKVT[:, :, 0:512].rearrange("g p t -> p g t"), [], [Braw])
        else:
            P.dma("sp", raw[:], QKVT[:, :, b * 512 - 3:(b + 1) * 512].rearrange("g p t -> p g t"), [], [Braw])
        zt, Bz = ztr.next()
        P.dma("sp", zt[:], TMZ[b * 512:(b + 1) * 512, 0:256].rearrange("(n p) c -> p n c", p=64), [], [Bz])
        P.op("act", lambda e, zt=zt: e.activation(out=zt[:], in_=zt[:], func=AF.Silu), [Bz], [Bz])
        P.op("pool", lambda e, zt=zt: e.tensor_tensor(out=zt[:], in0=zt[:], in1=nrep[:], op=ALU.mult), [Bz, Bc], [Bz])
        cs, Bcs = csr.next()
        qkn, Bqkn = qknr.next()
        for g in range(6):
            acc, Bacc = accr.next()
            eng = "dve"
            P.op("pool", lambda e, acc=acc, raw=raw, g=g: e.tensor_scalar(out=acc[:], in0=raw[:, g, 3:515], scalar1=cw[:, g, 3:4],
                                                                   scalar2=None, op0=ALU.mult), [Braw, Bc], [Bacc])
            for j in range(3):
                P.op(eng, lambda e, acc=acc, raw=raw, g=g, j=j: e.scalar_tensor_tensor(
                    out=acc[:], in0=raw[:, g, j:j + 512], scalar=cw[:, g, j:j + 1], in1=acc[:], op0=ALU.mult, op1=ALU.add),
                    [Braw, Bc, Bacc], [Bacc])
            P.op("act", lambda e, acc=acc, cs=cs, g=g: e.activation(out=cs[:, g, :], in_=acc[:], func=AF.Silu), [Bacc], [Bcs])
            if g < 4:
                sq, Bsq = sqr.next()
                P.op("pool", lambda e, sq=sq, cs=cs, g=g: e.tensor_tensor(out=sq[:], in0=cs[:, g, :], in1=cs[:, g, :], op=ALU.mult),
                     [Bcs], [Bsq])
                P.mm(banks[0][:, :], ones, sq[:], True, True, [Bc, Bsq], [Bssq])
                rn, Brn = rnr.next()
                P.op("dve", lambda e, rn=rn: e.tensor_scalar(out=rn[:], in0=banks[0][:, :], scalar1=NORM_EPS, scalar2=None,
                                                          op0=ALU.add), [Bssq], [Brn])
                P.op("act", lambda e, rn=rn: e.activation(out=rn[:], in_=rn[:], func=AF.Sqrt), [Brn], [Brn])
                P.op("dve", lambda e, rn=rn: e.reciprocal(out=rn[:], in_=rn[:]), [Brn], [Brn])
                sc = (128.0 ** -0.5) if g < 2 else 1.0
                P.op("dve", lambda e, rn=rn, cs=cs, qkn=qkn, g=g, sc=sc: e.scalar_tensor_tensor(
                    out=qkn[:, g, :], in0=cs[:, g, :], scalar=sc, in1=rn[:], op0=ALU.mult, op1=ALU.mult), [Bcs, Brn], [Bqkn])
        ob, Bob = obr.next()
        if STAGE < 3:
            continue
        for c in range(8):
            n = b * 8 + c
            sl = slice(c * 64, (c + 1) * 64)
            for h in range(2):
                qT = qkn[:, h, sl]
                kT = qkn[:, 2 + h, sl]
                vT = cs[:, 4 + h, sl]
                col = lambda t, n=n, h=h: t[:, n, h:h + 1]
                tk, Btk = r_tk.next()
                P.mm(tk[:, 0:128], kT, ident, True, True, [Bqkn, Bc], [Btk])
                P.mm(tk[:, 128:256], vT, ident, True, True, [Bcs, Bc], [Btk])
                kbg, Bkbg = s_kbg.next()
                kdec, Bkdec = s_kdec.next()
                vb, Bvb = s_vb.next()
                P.op("dve", lambda e, kbg=kbg, tk=tk, col=col: e.tensor_scalar(out=kbg[0:64, :], in0=tk[:, 0:128], scalar1=col(bexp),
                                                                            scalar2=None, op0=ALU.mult), [Btk, Bpre], [Bkbg])
                P.op("dve", lambda e, kdec=kdec, tk=tk, col=col: e.tensor_scalar(out=kdec[0:64, :], in0=tk[:, 0:128], scalar1=col(edec),
                                                                              scalar2=None, op0=ALU.mult), [Btk, Bpre], [Bkdec])
                P.op("dve", lambda e, vb=vb, tk=tk, col=col: e.tensor_scalar(out=vb[:], in0=tk[:, 128:256], scalar1=col(beta),
                                                                          scalar2=None, op0=ALU.mult), [Btk, Bpre], [Bvb])
                gbt, Bgbt = s_gbt.next()
                P.op("pool", lambda e, gbt=gbt, col=col: e.tensor_scalar(out=gbt[:], in0=ones[0:64, 0:64], scalar1=col(gg),
                                                                      scalar2=None, op0=ALU.mult), [Bc, Bpre], [Bgbt])
                gr, Bgr = r_gr.next()
                P.mm(gr, gbt[:], tri2, True, True, [Bgbt, Bc], [Bgr])
                kq, Bkq = r_kq.next()
                P.mm(kq[:, 0:64], kT, kT, True, True, [Bqkn], [Bkq])
                P.mm(kq[:, 64:128], qT, kT, True, True, [Bqkn], [Bkq])
                eD, BeD = s_eD.next()
                P.op("act", lambda e, eD=eD, gr=gr, col=col: e.activation(out=eD[:], in_=gr, func=AF.Exp, bias=col(Gc), scale=-1.0),
                     [Bgr, Bpre], [BeD])
                Dm, BDm = s_Dm.next()
                P.op("dve", lambda e, Dm=Dm, eD=eD: e.scalar_tensor_tensor(out=Dm[:], in0=eD[:], scalar=1.0, in1=mask2,
                                                                        op0=ALU.min, op1=ALU.mult), [BeD, Bc], [BDm])
                X, BX = s_X.next()
                P.op("dve", lambda e, X=X, kq=kq, Dm=Dm: e.tensor_tensor(out=X[:], in0=kq, in1=Dm[:], op=ALU.mult), [Bkq, BDm], [BX])
                UL, BUL = s_UL.next()
                P.op("dve", lambda e, UL=UL, X=X, col=col: e.scalar_tensor_tensor(
                    out=UL[:, 64:128], in0=X[:, 0:64], scalar=col(beta), in1=strict, op0=ALU.mult, op1=ALU.mult),
                    [BX, Bpre, Bc], [BUL])
                tt, Btt = r_tt.next()
                P.mm(tt[:, 0:64], UL[:, 64:128], id64, True, True, [BUL, Bc], [Btt])
                P.mm(tt[:, 64:128], X[:, 64:128], id64, True, True, [BX, Bc], [Btt])
                iT, BiT = s_iT.next()
                P.op("act", lambda e, UL=UL, tt=tt: e.copy(out=UL[:, 0:64], in_=tt[:, 0:64]), [Btt], [BUL])
                P.op("act", lambda e, iT=iT, tt=tt: e.copy(out=iT[:], in_=tt[:, 64:128]), [Btt], [BiT])
                PQ, BPQ = s_PQ.next()
                P.op("pool", lambda e, PQ=PQ, UL=UL: e.tensor_tensor(out=PQ[0:64, :], in0=i2t[:], in1=UL[:], op=ALU.subtract),
                     [BI2, BUL], [BPQ])
                for k in range(1, 6):
                    last = (k == 5)
                    sqp, Bsqp = r_sq.next()
                    P.mm(sqp[:, 0:64], UL[:, 64:128], UL[:, 0:64], True, True, [BUL], [Bsqp])
                    if not last:
                        P.mm(sqp[:, 64:128], UL[:, 0:64], UL[:, 64:128], True, True, [BUL], [Bsqp])
                    UL2, BUL2 = s_UL.next()
                    w_ = 64 if last else 128
                    P.op("act", lambda e, UL2=UL2, sqp=sqp, w_=w_: e.copy(out=UL2[:, 0:w_], in_=sqp[:, 0:w_]), [Bsqp], [BUL2])
                    pq, Bpq = r_pq.next()
                    P.mm(pq[:, 0:64], PQ[0:64, 64:128], UL2[:, 0:64], True, True, [BPQ, BUL2], [Bpq])
                    if not last:
                        P.mm(pq[:, 64:128], PQ[0:64, 0:64], UL2[:, 64:128], True, True, [BPQ, BUL2], [Bpq])
                    PQ2, BPQ2 = s_PQ.next()
                    P.op("dve", lambda e, PQ2=PQ2, PQ=PQ, pq=pq, w_=w_: e.tensor_tensor(out=PQ2[0:64, 0:w_], in0=PQ[0:64, 0:w_], in1=pq[:, 0:w_],
                                                                                     op=ALU.add), [BPQ, Bpq], [BPQ2])
                    UL, BUL, PQ, BPQ = UL2, BUL2, PQ2, BPQ2
                if STAGE < 4:
                    continue
                AiT = PQ[0:64, 0:64]
                wt, Bwt = r_wt.next()
                P.mm(wt, kbg[:], PQ[:, 0:64], True, True, [Bkbg, BPQ], [Bwt])
                if STAGE == 410:
                    continue
                wT, BwT = s_wT.next()
                P.op("act", lambda e, wT=wT, wt=wt: e.copy(out=wT[:], in_=wt), [Bwt], [BwT])
                if STAGE == 411:
                    continue
                up, Bup = r_u.next()
                P.mm(up, AiT, vb[:], True, True, [BPQ, Bvb], [Bup])
                if STAGE == 412:
                    continue
                u_, Bu_ = s_u.next()
                P.op("act", lambda e, u_=u_, up=up: e.copy(out=u_[:], in_=up), [Bup], [Bu_])
                if STAGE == 41:
                    continue
                Sc, BSc = S[h][scur[h]], BS[h][scur[h]]
                Sn, BSn = S[h][1 - scur[h]], BS[h][1 - scur[h]]
                ws, Bws = r_ws.next()
                P.mm(ws[:, 0:128], wT[:], Sc[:], True, True, [BwT, BSc], [Bws])
                P.mm(ws[:, 128:256], qT, Sc[:], True, True, [Bqkn, BSc], [Bws])
                vn, Bvn = s_vn.next()
                P.op("dve", lambda e, vn=vn, u_=u_, ws=ws: e.tensor_tensor(out=vn[0:64, :], in0=u_[:], in1=ws[:, 0:128], op=ALU.subtract),
                     [Bu_, Bws], [Bvn])
                oq, Boq = s_oq.next()
                P.op("dve", lambda e, oq=oq, ws=ws, col=col: e.tensor_scalar(out=oq[:], in0=ws[:, 128:256], scalar1=col(expG),
                                                                          scalar2=None, op0=ALU.mult), [Bws, Bpre], [Boq])
                if STAGE == 42:
                    continue
                iv, Biv = r_iv.next()
                P.mm(iv, iT[:], vn[0:64, :], True, True, [BiT, Bvn], [Biv])
                kv, Bkv = r_kv.next()
                P.mm(kv, kdec[:], vn[:], True, True, [Bkdec, Bvn], [Bkv])
                P.op("dve", lambda e, ob=ob, oq=oq, iv=iv, c=c, h=h: e.tensor_tensor(out=ob[:, c * 2 + h, :], in0=oq[:], in1=iv, op=ALU.add),
                     [Boq, Biv], [Bob])
                if STAGE == 43:
                    scur[h] = 1 - scur[h]
                    continue
                P.op("pool", lambda e, Sn=Sn, Sc=Sc, n=n, h=h: e.tensor_scalar(out=Sn[:], in0=Sc[:], scalar1=eGl[:, n, h:h + 1], scalar2=None,
                                                                            op0=ALU.mult), [BSc, Bpre], [BSn])
                P.op("dve", lambda e, Sn=Sn, kv=kv: e.tensor_tensor(out=Sn[:], in0=kv, in1=Sn[:], op=ALU.add), [Bkv, BSn], [BSn])
                scur[h] = 1 - scur[h]
        if STAGE < 5:
            continue
        P.op("pool", lambda e, ob=ob: e.tensor_tensor(out=osq[:], in0=ob[:], in1=ob[:], op=ALU.mult), [Bob], [Bosq])
        ns, Bns = nst.next()
        P.op("dve", lambda e, ns=ns: e.reduce_sum(out=ns[:, 0:16], in_=osq[:], axis=AX.X), [Bosq], [Bns])
        P.op("dve", lambda e, ns=ns: e.tensor_scalar(out=ns[:, 16:32], in0=ns[:, 0:16], scalar1=1.0 / 128.0, scalar2=NORM_EPS,
                                                  op0=ALU.mult, op1=ALU.add), [Bns], [Bns])
        P.op("act", lambda e, ns=ns: e.activation(out=ns[:, 16:32], in_=ns[:, 16:32], func=AF.Sqrt), [Bns], [Bns])
        P.op("dve", lambda e, ns=ns: e.reciprocal(out=ns[:, 16:32], in_=ns[:, 16:32]), [Bns], [Bns])
        og, Bog = ogr.next()
        for c in range(8):
            for h in range(2):
                P.op("dve", lambda e, og=og, ob=ob, ns=ns, zt=zt, c=c, h=h: e.scalar_tensor_tensor(
                    out=og[:, c, h * 128:(h + 1) * 128], in0=ob[:, c * 2 + h, :], scalar=ns[:, 16 + c * 2 + h:17 + c * 2 + h],
                    in1=zt[:, c, h * 128:(h + 1) * 128], op0=ALU.mult, op1=ALU.mult), [Bob, Bns, Bz], [Bog])
        P.dma("sp", oA[b * 512:(b + 1) * 512, :].rearrange("(n p) c -> p n c", p=64), og[:], [Bog], [dor.next()])
    cx.finish()


def k1_consts():
    c = np.zeros((128, 5, 128), np.float32)
    c[:, 0, :] = np.eye(128)
    c[:, 1, :] = 1.0
    p = np.arange(64)[:, None]; i = np.arange(64)[None, :]
    tri_le = (p <= i).astype(np.float32)
    c[0:64, 2, 0:64] = tri_le; c[0:64, 2, 64:128] = tri_le
    tril = (i <= p).astype(np.float32)
    c[0:64, 3, 0:64] = tril; c[0:64, 3, 64:128] = tril
    c[0:64, 4, 0:64] = (i < p).astype(np.float32)
    c[0:64, 4, 64:128] = np.eye(64)
    return c


def phase_p3(nc, oAT, oBT, gates, w_up_a, w_up_b, w_o, xT, modb, adat, rT, t0, TB=512):
    cx = Ctx(nc)
    P = cx.P
    mod, Bm = load_mod(cx, modb, adat, [])
    ts = slice(t0, t0 + TB)
    oa = cx.sb([128, 16, TB], BF16, "oa")
    ob = cx.sb([128, 16, TB], BF16, "ob")
    Boa, Bob = cx.buf("oa"), cx.buf("ob")
    P.dma("pool", oa[:], oAT[:, :, ts].rearrange("k p t -> p k t"), [], [Boa])
    P.dma("pool", ob[:], oBT[:, :, ts].rearrange("k p t -> p k t"), [], [Bob])
    mg = cx.sb([128, KC, TB], BF16, "mg")
    Bmg = [cx.buf(f"mg{m}") for m in range(KC)]
    wra = sb_ring(cx, 2, [128, 16, 256], BF16, "wa")
    wrb = sb_ring(cx, 2, [128, 16, 256], BF16, "wb")
    psr = ps_ring(cx, 4, [128, TB], "ps")
    gr = sb_ring(cx, 4, [128, TB], BF16, "g")
    t1r = sb_ring(cx, 2, [128, TB], F32, "t1")
    t2r = sb_ring(cx, 2, [128, TB], F32, "t2")
    for w2 in range(16):
        wa, Bwa = wra.next()
        wb, Bwb = wrb.next()
        P.dma("pool", wa[:], w_up_a[:, w2 * 256:(w2 + 1) * 256].rearrange("(kc p) j -> p kc j", p=128), [], [Bwa])
        P.dma("pool", wb[:], w_up_b[:, w2 * 256:(w2 + 1) * 256].rearrange("(kc p) j -> p kc j", p=128), [], [Bwb])
        for mi in range(2):
            m = w2 * 2 + mi
            ga, Bga = gr.next()
            gb, Bgb = gr.next()
            P.dma("sp", ga[:], gates[m, :, ts], [], [Bga])
            P.dma("sp", gb[:], gates[32 + m, :, ts], [], [Bgb])
            pa, Bpa = psr.next()
            for kc in range(16):
                P.mm(pa[:], wa[:, kc, mi * 128:(mi + 1) * 128], oa[:, kc, :], kc == 0, kc == 15, [Bwa, Boa], [Bpa])
            pb, Bpb = psr.next()
            for kc in range(16):
                P.mm(pb[:], wb[:, kc, mi * 128:(mi + 1) * 128], ob[:, kc, :], kc == 0, kc == 15, [Bwb, Bob], [Bpb])
            t1, Bt1 = t1r.next()
            t2, Bt2 = t2r.next()
            P.op("dve", lambda e, t1=t1, pa=pa, ga=ga: e.tensor_tensor(out=t1[:], in0=pa[:], in1=ga[:], op=ALU.mult), [Bpa, Bga], [Bt1])
            P.op("dve", lambda e, t2=t2, pb=pb, gb=gb: e.tensor_tensor(out=t2[:], in0=pb[:], in1=gb[:], op=ALU.mult), [Bpb, Bgb], [Bt2])
            P.op("pool", lambda e, t1=t1, t2=t2, m=m: e.tensor_tensor(out=mg[:, m, :], in0=t1[:], in1=t2[:], op=ALU.add), [Bt1, Bt2], [Bmg[m]])
    wring = sb_ring(cx, 2, [128, KC, 256], BF16, "wo")
    xr = sb_ring(cx, 3, [128, TB], F32, "x")
    rr = sb_ring(cx, 3, [128, TB], F32, "r")
    dor = Ring([cx.buf(f"do{i}") for i in range(3)])

    def epi(m, ps, Bp):
        xt, Bx = xr.next()
        P.dma("sp", xt[:], xT[m, :, ts], [], [Bx])
        P.op("act", lambda e, xt=xt: e.mul(out=xt[:], in_=xt[:], mul=DN_ALPHA), [Bx], [Bx])
        rt, Br = rr.next()
        P.op("dve", lambda e, rt=rt, ps=ps, xt=xt, m=m: e.scalar_tensor_tensor(
            out=rt[:], in0=ps[:], scalar=mod[:, 2, m:m + 1], in1=xt[:], op0=ALU.mult, op1=ALU.add), [Bp, Bx, Bm], [Br])
        P.dma("sp", rT[m, :, ts], rt[:], [Br], [dor.next()])
    stream_linear(cx, w_o, 0, KC, KC, lambda kc, a, b: (mg[:, kc, a:b], [Bmg[kc]]), TB, wring, psr, epi)
    cx.finish()


def phase_ln(nc, rT, lng, lnb, modb, adat, sc_idx, xoT, uoT, cstk, TL, TB=512):
    cx = Ctx(nc)
    P = cx.P
    mod, Bm = load_mod(cx, modb, adat, [sc_idx + 1])
    g = cx.sb([128, KC], F32, "lng")
    b = cx.sb([128, KC], F32, "lnb")
    cs_ = cx.sb([128, 5, 128], F32, "cst")
    Bc = cx.buf("c")
    P.dma("sp", g[:], lng, [], [Bc])
    P.dma("sp", b[:], lnb, [], [Bc])
    P.dma("sp", cs_[:], cstk, [], [Bc])
    ones = cs_[:, 1, :]
    rbuf = sb_ring(cx, 2, [128, KC, TB], F32, "r")
    sqr = sb_ring(cx, 3, [128, TB], F32, "sq")
    ps1 = ps_ring(cx, 2, [128, TB], "s1")
    ps2 = ps_ring(cx, 2, [128, TB], "s2")
    mr = sb_ring(cx, 2, [128, TB], F32, "mean")
    vr = sb_ring(cx, 2, [128, TB], F32, "var")
    tr = sb_ring(cx, 3, [128, TB], F32, "t")
    xor_ = sb_ring(cx, 3, [128, TB], F32, "xo")
    uor = sb_ring(cx, 3, [128, TB], BF16, "uo")
    dor = Ring([cx.buf(f"do{i}") for i in range(4)])
    for tb in range(TL // TB):
        ts = slice(tb * TB, (tb + 1) * TB)
        r, Br = rbuf.next()
        P.dma("sp", r[:], rT[:, :, ts].rearrange("k p t -> p k t"), [], [Br])
        s1, Bs1 = ps1.next()
        s2, Bs2 = ps2.next()
        for kc in range(KC):
            sq, Bsq = sqr.next()
            P.op("pool", lambda e, sq=sq, r=r, kc=kc: e.tensor_tensor(out=sq[:], in0=r[:, kc, :], in1=r[:, kc, :], op=ALU.mult), [Br], [Bsq])
            P.mm(s1[:], ones, r[:, kc, :], kc == 0, kc == KC - 1, [Bc, Br], [Bs1])
            P.mm(s2[:], ones, sq[:], kc == 0, kc == KC - 1, [Bc, Bsq], [Bs2])
        mean, Bmean = mr.next()
        var, Bvar = vr.next()
        P.op("dve", lambda e, mean=mean, s1=s1: e.tensor_scalar(out=mean[:], in0=s1[:], scalar1=1.0 / D, scalar2=None, op0=ALU.mult), [Bs1], [Bmean])
        P.op("dve", lambda e, var=var, s2=s2: e.tensor_scalar(out=var[:], in0=s2[:], scalar1=1.0 / D, scalar2=LN_EPS, op0=ALU.mult, op1=ALU.add), [Bs2], [Bvar])
        sq, Bsq = sqr.next()
        P.op("pool", lambda e, sq=sq, mean=mean: e.tensor_tensor(out=sq[:], in0=mean[:], in1=mean[:], op=ALU.mult), [Bmean], [Bsq])
        P.op("dve", lambda e, var=var, sq=sq: e.tensor_tensor(out=var[:], in0=var[:], in1=sq[:], op=ALU.subtract), [Bvar, Bsq], [Bvar])
        P.op("act", lambda e, var=var: e.activation(out=var[:], in_=var[:], func=AF.Sqrt), [Bvar], [Bvar])
        P.op("dve", lambda e, var=var: e.reciprocal(out=var[:], in_=var[:]), [Bvar], [Bvar])
        for kc in range(KC):
            t, Bt = tr.next()
            P.op("pool", lambda e, t=t, r=r, mean=mean, kc=kc: e.tensor_tensor(out=t[:], in0=r[:, kc, :], in1=mean[:], op=ALU.subtract), [Br, Bmean], [Bt])
            P.op("dve", lambda e, t=t, var=var: e.tensor_tensor(out=t[:], in0=t[:], in1=var[:], op=ALU.mult), [Bt, Bvar], [Bt])
            xo, Bxo = xor_.next()
            P.op("act", lambda e, xo=xo, t=t, kc=kc: e.activation(out=xo[:], in_=t[:], func=AF.Identity, bias=b[:, kc:kc + 1],
                                                                scale=g[:, kc:kc + 1]), [Bt, Bc], [Bxo])
            P.dma("sp", xoT[kc, :, ts], xo[:], [Bxo], [dor.next()])
            uo, Buo = uor.next()
            P.op("dve", lambda e, uo=uo, xo=xo, kc=kc: e.tensor_scalar(
                out=uo[:], in0=xo[:], scalar1=mod[:, sc_idx + 1, kc:kc + 1], scalar2=mod[:, sc_idx, kc:kc + 1],
                op0=ALU.mult, op1=ALU.add), [Bxo, Bm], [Buo])
            P.dma("sp", uoT[kc, :, ts], uo[:], [Buo], [dor.next()])
    cx.finish()


def phase_moe(nc, uT, xT, w_router, brep, w_gate_up, bgu, w_down, b_down, modb, adat, cstk, eye32, rT, t0, TB=512):
    cx = Ctx(nc)
    P = cx.P
    mod, Bm = load_mod(cx, modb, adat, [])
    ts = slice(t0, t0 + TB)
    NT = TB // 128
    Bc = cx.buf("c")
    cs_ = cx.sb([128, 5, 128], F32, "cst")
    ey = cx.sb([32, 32], F32, "eye")
    br = cx.sb([128, 32], F32, "brep")
    bg = cx.sb([128, NE, 4], F32, "bgu")
    wr = cx.sb([128, KC, NE], BF16, "wr")
    u = cx.sb([128, KC, TB], BF16, "u")
    Bu = cx.buf("u")
    P.dma("sp", cs_[:], cstk, [], [Bc])
    P.dma("sp", ey[:], eye32, [], [Bc])
    P.dma("sp", br[:], brep, [], [Bc])
    P.dma("sp", bg[:], bgu, [], [Bc])
    Bw0 = cx.buf("w0")
    P.dma("pool", wr[:], w_router.rearrange("(kc p) e -> p kc e", p=128), [], [Bw0])
    P.dma("sp", u[:], uT[:, :, ts].rearrange("k p t -> p k t"), [], [Bu])
    ident = cs_[:, 0, :]
    ones = cs_[:, 1, :]
    combT = cx.sb([32, TB], F32, "combT")
    combTb = cx.sb([32, TB], BF16, "combTb")
    BcT = cx.buf("combT")
    psl = ps_ring(cx, 2, [128, 128], "psl")
    lg = cx.sb([128, NE], F32, "lg")
    cur = cx.sb([128, NE], F32, "cur")
    eq = cx.sb([128, NE], F32, "eq")
    pe_ = cx.sb([128, NE], F32, "pe")
    mx = cx.sb([128, 8], F32, "mx")
    Brt = cx.buf("rt")
    for tt in range(NT):
        pl, Bpl = psl.next()
        for kc in range(KC):
            P.mm(pl[:, 0:NE], u[:, kc, tt * 128:(tt + 1) * 128], wr[:, kc, :], kc == 0, kc == KC - 1, [Bu, Bw0], [Bpl])
        P.op("dve", lambda e, pl=pl: e.tensor_tensor(out=lg[:], in0=pl[:, 0:NE], in1=br[:], op=ALU.add), [Bpl, Bc], [Brt])
        P.op("pool", lambda e: e.tensor_copy(out=cur[:], in_=lg[:]), [Brt], [Brt])
        for i in range(4):
            P.op("dve", lambda e, i=i: e.reduce_max(out=mx[:, i:i + 1], in_=cur[:], axis=AX.X), [Brt], [Brt])
            if i < 3:
                P.op("dve", lambda e, i=i: e.tensor_scalar(out=eq[:], in0=cur[:], scalar1=mx[:, i:i + 1], scalar2=None, op0=ALU.is_equal), [Brt], [Brt])
                P.op("dve", lambda e: e.scalar_tensor_tensor(out=cur[:], in0=eq[:], scalar=-1e30, in1=cur[:], op0=ALU.mult, op1=ALU.add), [Brt], [Brt])
        P.op("dve", lambda e: e.tensor_scalar(out=eq[:], in0=lg[:], scalar1=mx[:, 3:4], scalar2=None, op0=ALU.is_ge), [Brt], [Brt])
        P.op("dve", lambda e: e.tensor_scalar(out=mx[:, 4:5], in0=mx[:, 0:1], scalar1=-1.0, scalar2=None, op0=ALU.mult), [Brt], [Brt])
        P.op("act", lambda e: e.activation(out=pe_[:], in_=lg[:], func=AF.Exp, bias=mx[:, 4:5], scale=1.0), [Brt], [Brt])
        P.op("dve", lambda e: e.tensor_tensor(out=pe_[:], in0=pe_[:], in1=eq[:], op=ALU.mult), [Brt], [Brt])
        P.op("dve", lambda e: e.reduce_sum(out=mx[:, 5:6], in_=pe_[:], axis=AX.X), [Brt], [Brt])
        P.op("dve", lambda e: e.reciprocal(out=mx[:, 6:7], in_=mx[:, 5:6]), [Brt], [Brt])
        P.op("dve", lambda e: e.tensor_scalar(out=pe_[:], in0=pe_[:], scalar1=mx[:, 6:7], scalar2=None, op0=ALU.mult), [Brt], [Brt])
        pl2, Bpl2 = psl.next()
        P.mm(pl2[0:NE, :], pe_[:], ident, True, True, [Brt, Bc], [Bpl2])
        P.op("act", lambda e, pl2=pl2, tt=tt: e.copy(out=combT[:, tt * 128:(tt + 1) * 128], in_=pl2[0:NE, :]), [Bpl2], [BcT])
    P.op("act", lambda e: e.copy(out=combTb[:], in_=combT[:]), [BcT], [BcT])
    hall = cx.sb([128, 2 * NE, TB], BF16, "hall")
    Bh = [cx.buf(f"h{e}") for e in range(NE)]
    wgr = sb_ring(cx, 2, [128, KC, 128], BF16, "wg")
    psg = ps_ring(cx, 4, [128, TB], "psg")
    psb = ps_ring(cx, 2, [128, TB], "psb")
    cmr = sb_ring(cx, 2, [32, TB], F32, "cm")
    f32r = {n: sb_ring(cx, 2, [128, TB], F32, n) for n in ("gp", "sg", "up")}
    for ex in range(NE):
        cm, Bcm = cmr.next()
        P.op("pool", lambda e, cm=cm, ex=ex: e.tensor_scalar(out=cm[:], in0=combT[:], scalar1=ey[:, ex:ex + 1], scalar2=None, op0=ALU.mult),
             [BcT, Bc], [Bcm])
        pb, Bpb = psb.next()
        P.mm(pb[:], ones[0:32, :], cm[:], True, True, [Bc, Bcm], [Bpb])
        pss = []
        for fc in range(4):
            wg, Bwg = wgr.next()
            P.dma("pool", wg[:], w_gate_up[ex, :, fc * 128:(fc + 1) * 128].rearrange("(kc p) j -> p kc j", p=128), [], [Bwg])
            ps, Bp = psg.next()
            for kc in range(KC):
                P.mm(ps[:], wg[:, kc, :], u[:, kc, :], kc == 0, kc == KC - 1, [Bwg, Bu], [Bp])
            pss.append((ps, Bp))
        for j in range(2):
            (pg, Bpg), (pu, Bpu) = pss[j], pss[2 + j]
            gp, Bgp = f32r["gp"].next()
            sg, Bsg = f32r["sg"].next()
            up, Bup = f32r["up"].next()
            t1, Bt1 = sg, Bsg
            t2, Bt2 = up, Bup
            P.op("dve", lambda e, gp=gp, pg=pg, ex=ex, j=j: e.tensor_scalar(out=gp[:], in0=pg[:], scalar1=bg[:, ex, j:j + 1], scalar2=7.0,
                                                                         op0=ALU.add, op1=ALU.min), [Bpg, Bc], [Bgp])
            P.op("act", lambda e, sg=sg, gp=gp: e.activation(out=sg[:], in_=gp[:], func=AF.Sigmoid, scale=1.702), [Bgp], [Bsg])
            P.op("dve", lambda e, up=up, pu=pu, ex=ex, j=j: e.tensor_scalar(out=up[:], in0=pu[:], scalar1=bg[:, ex, 2 + j:3 + j], scalar2=7.0,
                                                                         op0=ALU.add, op1=ALU.min), [Bpu, Bc], [Bup])
            P.op("pool", lambda e, up=up: e.tensor_scalar(out=up[:], in0=up[:], scalar1=-7.0, scalar2=1.0, op0=ALU.max, op1=ALU.add), [Bup], [Bup])
            P.op("pool", lambda e, t1=t1, gp=gp, sg=sg: e.tensor_tensor(out=t1[:], in0=gp[:], in1=sg[:], op=ALU.mult), [Bgp, Bsg], [Bt1])
            P.op("dve", lambda e, t2=t2, up=up, pb=pb: e.tensor_tensor(out=t2[:], in0=up[:], in1=pb[:], op=ALU.mult), [Bup, Bpb], [Bt2])
            P.op("pool", lambda e, t1=t1, t2=t2, ex=ex, j=j: e.tensor_tensor(out=hall[:, ex * 2 + j, :], in0=t1[:], in1=t2[:], op=ALU.mult),
                 [Bt1, Bt2], [Bh[ex]])
    wdr = sb_ring(cx, 2, [128, 2 * NE, 128], BF16, "wd")
    xr = sb_ring(cx, 2, [128, TB], F32, "x")
    rr = sb_ring(cx, 2, [128, TB], F32, "r")
    bdr = sb_ring(cx, 2, [32, 128], BF16, "bd")
    dor = Ring([cx.buf(f"do{i}") for i in range(3)])
    for m in range(KC):
        wd, Bwd = wdr.next()
        bd, Bbd = bdr.next()
        P.dma("pool", bd[:], b_down[:, m * 128:(m + 1) * 128], [], [Bbd])
        for half in range(2):
            P.dma("pool", wd[:, half * NE:(half + 1) * NE, :],
                  w_down[half * 16:(half + 1) * 16, :, m * 128:(m + 1) * 128].rearrange("e (fc p) j -> p (e fc) j", p=128), [], [Bwd])
        ps, Bp = psg.next()
        for i in range(2 * NE):
            P.mm(ps[:], wd[:, i, :], hall[:, i, :], i == 0, False, [Bwd, Bh[i // 2]], [Bp])
        P.mm(ps[:], bd[:], combTb[:], False, True, [Bbd, BcT], [Bp])
        xt, Bx = xr.next()
        P.dma("sp", xt[:], xT[m, :, ts], [], [Bx])
        P.op("act", lambda e, xt=xt: e.mul(out=xt[:], in_=xt[:], mul=DN_ALPHA), [Bx], [Bx])
        rt, Br = rr.next()
        P.op("dve", lambda e, rt=rt, ps=ps, xt=xt, m=m: e.scalar_tensor_tensor(
            out=rt[:], in0=ps[:], scalar=mod[:, 5, m:m + 1], in1=xt[:], op0=ALU.mult, op1=ALU.add), [Bp, Bx, Bm], [Br])
        P.dma("sp", rT[m, :, ts], rt[:], [Br], [dor.next()])
    cx.finish()


N_MOD = 6
T_ALL = 8192
TLOC = T_ALL // NCORE


def _pk(v):
    return np.ascontiguousarray(np.asarray(v, np.float32).reshape(KC, 128).T)


def _pk6(m):
    return np.ascontiguousarray(np.asarray(m, np.float32).reshape(N_MOD, KC, 128).transpose(2, 0, 1))


def _fm(a, k):
    return np.ascontiguousarray(a.T.reshape(k, 128, -1))


def _dt(nc, name, shape, dt, kind):
    return nc.dram_tensor(name, list(shape), dt, kind=kind).ap()


def _build_adaln(ncols):
    nc = bass.Bass("TRN2", target_bir_lowering=False)
    c_pk = _dt(nc, "c_pk", [128, KC], F32, "ExternalInput")
    w_sh = _dt(nc, "w_sh", [D, ncols], F32, "ExternalInput")
    b_sh = _dt(nc, "b_sh", [1, ncols], F32, "ExternalInput")
    o = _dt(nc, "mod_sh", [1, ncols], F32, "ExternalOutput")
    phase_adaln(nc, c_pk, w_sh, b_sh, o, ncols)
    return nc


def _build_p1():
    nc = bass.Bass("TRN2", target_bir_lowering=False)
    xT = _dt(nc, "xT", [KC, 128, TLOC], F32, "ExternalInput")
    modb = _dt(nc, "modb", [128, N_MOD, KC], F32, "ExternalInput")
    adat = _dt(nc, "adat", [128, N_MOD, KC], F32, "ExternalInput")
    w_g = _dt(nc, "w_g", [D, 8192], F32, "ExternalInput")
    uT_out = _dt(nc, "uT_out", [KC, 128, TLOC], BF16, "ExternalOutput")
    gates = _dt(nc, "gates", [64, 128, TLOC], BF16, "ExternalOutput")
    phase_p1(nc, xT, modb, adat, w_g, uT_out, gates, TLOC, gcol0=0)
    return nc


def _build_k1():
    T = T_ALL
    nc = bass.Bass("TRN2", target_bir_lowering=False)
    UT = _dt(nc, "UT", [KC, 128, T], BF16, "ExternalInput")
    Wc = _dt(nc, "Wc", [D, NWC], F32, "ExternalInput")
    convw = _dt(nc, "convw", [128, 6, 4], F32, "ExternalInput")
    hc = _dt(nc, "hc", [64, 4], F32, "ExternalInput")
    normrep = _dt(nc, "normrep", [64, 8, 256], F32, "ExternalInput")
    cst = _dt(nc, "cst", [128, 5, 128], F32, "ExternalInput")
    biasT = _dt(nc, "biasT", [4, 128, 256], F32, "ExternalInput")
    maskc = _dt(nc, "maskc", [2, 128, 256], F32, "ExternalInput")
    sinkb = _dt(nc, "sinkb", [128, 4], F32, "ExternalInput")
    ident = _dt(nc, "ident", [128, 128], F32, "ExternalInput")
    QKVT = _dt(nc, "QKVT", [6, 128, T], F32, "ExternalOutput")
    QBT = _dt(nc, "QBT", [5, 64, T], F32, "ExternalOutput")
    TMZ = _dt(nc, "TMZ", [T, NTM], F32, "ExternalOutput")
    oA = _dt(nc, "oA", [T, 256], F32, "ExternalOutput")
    oB = _dt(nc, "oB", [T, 256], F32, "ExternalOutput")
    phase_p2a(nc, UT, Wc, QKVT, QBT, TMZ, T)
    phase_p2c(nc, QBT, TMZ, biasT, maskc, sinkb, ident, oB, T)
    phase_p2b(nc, QKVT, TMZ, convw, hc, normrep, cst, oA, T)
    return nc


def _build_k2():
    TL = TLOC
    nc = bass.Bass("TRN2", target_bir_lowering=False)
    I = lambda n, s, d=F32: _dt(nc, n, s, d, "ExternalInput")
    O = lambda n, s, d=F32: _dt(nc, n, s, d, "ExternalOutput")
    oAT = I("oAT", [16, 128, TL]); oBT = I("oBT", [16, 128, TL]); gates = I("gates", [64, 128, TL], BF16)
    wa = I("w_up_a", [2048, D]); wb = I("w_up_b", [2048, D]); wo = I("w_o", [D, D])
    xT = I("xT", [KC, 128, TL]); modb = I("modb", [128, N_MOD, KC]); adat = I("adat", [128, N_MOD, KC])
    l1g = I("ln1g", [128, KC]); l1b = I("ln1b", [128, KC]); l2g = I("ln2g", [128, KC]); l2b = I("ln2b", [128, KC])
    cstk = I("cstk", [128, 5, 128]); eye32 = I("eye32", [32, 32])
    wr = I("w_router", [D, NE]); brep = I("brep", [128, NE]); wgu = I("w_gate_up", [NE, D, 2 * DE]); bgu = I("bgu", [128, NE, 4])
    wd = I("w_down", [NE, DE, D]); bdn = I("b_down", [NE, D])
    r1T = O("r1T", [KC, 128, TL]); x1T = O("x1T", [KC, 128, TL]); u2T = O("u2T", [KC, 128, TL], BF16)
    r2T = O("r2T", [KC, 128, TL]); x2T = O("x2T", [KC, 128, TL]); u3T = O("u3T", [KC, 128, TL], BF16)
    for t0 in range(0, TL, 512):
        phase_p3(nc, oAT, oBT, gates, wa, wb, wo, xT, modb, adat, r1T, t0, 512)
    phase_ln(nc, r1T, l1g, l1b, modb, adat, 3, x1T, u2T, cstk, TL)
    for t0 in range(0, TL, 512):
        phase_moe(nc, u2T, x1T, wr, brep, wgu, bgu, wd, bdn, modb, adat, cstk, eye32, r2T, t0, 512)
    phase_ln(nc, r2T, l2g, l2b, modb, adat, 0, x2T, u3T, cstk, TL)
    return nc


def _band_tables(rel_bias):
    import math
    i = np.arange(128)[:, None]
    j = np.arange(256)[None, :]
    d = i + 128 - j
    dc = np.clip(d, 0, 127)
    df = np.maximum(dc, 1).astype(np.float32)
    large = 16 + (np.log(df / 16) / math.log(128 / 16) * 16).astype(np.int32)
    large = np.minimum(large, 31)
    bucket = np.where(dc < 16, dc, large)
    bias_all = np.asarray(rel_bias, np.float32)[bucket]
    valid = (d >= 0) & (d < 128)
    m0 = np.where(valid, 0.0, -30000.0).astype(np.float32)
    m1 = m0.copy()
    m1[:, :128] = -30000.0
    return bias_all, np.stack([m0, m1])


def kernel(x, c, w_ada, b_ada, ada_table, rel_bias, w_in, conv_w, a_log, dt_bias, norm_a, sinks,
           w_up_a, w_up_b, w_o, ln1_g, ln1_b, w_router, b_router, w_gate_up, b_gate_up, w_down,
           b_down, ln2_g, ln2_b):
    f32 = np.float32
    cores = list(range(NCORE))
    x = np.asarray(x, f32)
    ncols = N_MOD * D // NCORE
    c_pk = _pk(np.asarray(c, f32)[0])
    nc0 = _build_adaln(ncols)
    in0 = [{"c_pk": c_pk,
            "w_sh": np.ascontiguousarray(w_ada[:, r * ncols:(r + 1) * ncols]),
            "b_sh": np.ascontiguousarray(np.asarray(b_ada, f32)[None, r * ncols:(r + 1) * ncols])} for r in cores]
    r0 = run_bass_kernel_spmd(nc0, in0, core_ids=cores)
    mod_base = np.concatenate([r0.results[r]["mod_sh"][0] for r in cores]).reshape(N_MOD, D)
    modb = _pk6(mod_base)
    bias_all, maskc = _band_tables(rel_bias)
    cst = k1_consts()
    ident = np.eye(128, dtype=f32)
    eye32 = np.eye(32, dtype=f32)
    nc1, nck1, nck2 = _build_p1(), _build_k1(), _build_k2()
    xT = [np.ascontiguousarray(x[0, r * TLOC:(r + 1) * TLOC, :].T.reshape(KC, 128, TLOC)) for r in cores]
    for l in range(4):
        adat = _pk6(ada_table[l])
        Wl = np.asarray(w_in[l], f32)
        w_g = np.ascontiguousarray(Wl[:, OFF_GA:OFF_GA + 8192])
        r1 = run_bass_kernel_spmd(nc1, [{"xT": xT[r], "modb": modb, "adat": adat, "w_g": w_g} for r in cores], core_ids=cores)
        UT = np.ascontiguousarray(np.concatenate([r1.results[r]["uT_out"] for r in cores], axis=2))
        gates = [r1.results[r]["gates"] for r in cores]
        del w_g
        cw = np.asarray(conv_w[l], f32)
        ins = []
        for hg in cores:
            cols = []
            for base in (OFF_QA, OFF_KA, OFF_VA):
                for h in range(2):
                    cols.append(np.arange(base + (2 * hg + h) * 128, base + (2 * hg + h + 1) * 128))
            cols.append(np.arange(OFF_QB + hg * 256, OFF_QB + (hg + 1) * 256))
            cols.append(np.arange(OFF_KB + hg * 64, OFF_KB + (hg + 1) * 64))
            cols.append(np.arange(OFF_ZA + hg * 256, OFF_ZA + (hg + 1) * 256))
            cols.append(np.arange(OFF_VB + hg * 64, OFF_VB + (hg + 1) * 64))
            cols.append(np.arange(OFF_BA + 2 * hg, OFF_BA + 2 * hg + 2))
            cols.append(np.arange(OFF_AA + 2 * hg, OFF_AA + 2 * hg + 2))
            cols = np.concatenate(cols)
            Wc = np.ascontiguousarray(Wl[:, cols])
            chb = [kind * 2048 + (2 * hg + h) * 128 for kind in range(3) for h in range(2)]
            convw = np.ascontiguousarray(np.stack([cw[:, b0:b0 + 128] for b0 in chb], 0).transpose(2, 0, 1))
            hcv = np.concatenate([np.asarray(a_log[l], f32)[2 * hg:2 * hg + 2], np.asarray(dt_bias[l], f32)[2 * hg:2 * hg + 2]])
            heads = [4 * hg + k for k in range(4)]
            ins.append({"UT": UT, "Wc": Wc, "convw": convw,
                        "hc": np.ascontiguousarray(np.broadcast_to(hcv[None], (64, 4))),
                        "normrep": np.ascontiguousarray(np.broadcast_to(np.tile(np.asarray(norm_a[l], f32), 2)[None, None], (64, 8, 256))),
                        "cst": cst, "biasT": np.ascontiguousarray(bias_all[:, :, heads].transpose(2, 0, 1)), "maskc": maskc,
                        "sinkb": np.ascontiguousarray(np.broadcast_to(np.asarray(sinks[l], f32)[heads][None], (128, 4))),
                        "ident": ident})
        rk1 = run_bass_kernel_spmd(nck1, ins, core_ids=cores)
        oA = np.concatenate([rk1.results[r]["oA"] for r in cores], axis=1)
        oB = np.concatenate([rk1.results[r]["oB"] for r in cores], axis=1)
        del ins, rk1, UT
        shared = {"w_up_a": np.asarray(w_up_a[l], f32), "w_up_b": np.asarray(w_up_b[l], f32), "w_o": np.asarray(w_o[l], f32),
                  "modb": modb, "adat": adat, "ln1g": _pk(ln1_g[l]), "ln1b": _pk(ln1_b[l]), "ln2g": _pk(ln2_g[l]), "ln2b": _pk(ln2_b[l]),
                  "cstk": cst, "eye32": eye32, "w_router": np.asarray(w_router[l], f32),
                  "brep": np.ascontiguousarray(np.broadcast_to(np.asarray(b_router[l], f32)[None], (128, NE))),
                  "w_gate_up": np.asarray(w_gate_up[l], f32),
                  "bgu": np.ascontiguousarray(np.asarray(b_gate_up[l], f32).reshape(NE, 4, 128).transpose(2, 0, 1)),
                  "w_down": np.asarray(w_down[l], f32), "b_down": np.asarray(b_down[l], f32)}
        ins = []
        for r in cores:
            sl = slice(r * TLOC, (r + 1) * TLOC)
            d_ = dict(shared)
            d_.update({"oAT": _fm(oA[sl], 16), "oBT": _fm(oB[sl], 16), "gates": gates[r], "xT": xT[r]})
            ins.append(d_)
        rk2 = run_bass_kernel_spmd(nck2, ins, core_ids=cores)
        xT = [np.ascontiguousarray(rk2.results[r]["x2T"]) for r in cores]
        del ins, rk2, oA, oB, gates
    out = np.concatenate([xT[r].reshape(D, TLOC).T for r in cores], axis=0)
    return np.ascontiguousarray(out[None]).astype(np.float32)
```
